# Optimizing a Trainium2 kernel written in Bass

```python
import math
import jax, jax.numpy as jnp
from jax import lax
import numpy as np

D_MODEL = 1024
BATCH = 8
SEQ = 2048
DEPTH = 4
DEC_BATCH = 128
DEC_SEQ = 4
PAST_LEN = 16384
PAGE_SIZE = 128

MIX_WIDTH = D_MODEL
RWKV_WIDTH = MIX_WIDTH // 2
RWKV_HEAD = 64
RWKV_HEADS = RWKV_WIDTH // RWKV_HEAD
LORA_W = 64
LORA_A = 64
LORA_V = 32
S5_WIDTH = MIX_WIDTH - RWKV_WIDTH
S5_GROUP = 16
S5_GROUPS = S5_WIDTH // S5_GROUP
S5_STATE = 64
MEM_LEN = 256
X_HEADS = 4
X_HEAD_DIM = D_MODEL // X_HEADS
SHIFT_COLS = 3 * RWKV_WIDTH + LORA_W + LORA_A
IN_COLS = SHIFT_COLS + RWKV_WIDTH + 2 * S5_WIDTH
NORM_EPS = 1e-6
GN_EPS = 64e-5

kernel_name = 'hymba_rwkv7_s5_xmem_step'

F32 = jnp.float32


def rms_norm(x, g):
    xf = x.astype(F32)
    y = xf * lax.rsqrt(jnp.mean(xf * xf, axis=-1, keepdims=True) + NORM_EPS)
    return (y * g.astype(F32)).astype(x.dtype)


def token_shift(p, prev, mu):
    p_prev = jnp.concatenate([prev[:, None, :].astype(p.dtype), p[:, :-1, :]], axis=1)
    return p + (p_prev - p) * mu


def wkv_scan(r, w, k, v, kk, a, S0):
    def step(S, inp):
        r_t, w_t, k_t, v_t, kk_t, a_t = inp
        sk = jnp.einsum('bhij,bhj->bhi', S, kk_t)
        S = (S * w_t[:, :, None, :]
             - sk[..., None] * (kk_t * a_t)[:, :, None, :]
             + v_t[..., None] * k_t[:, :, None, :])
        y = jnp.einsum('bhij,bhj->bhi', S, r_t)
        return S, y
    xs = tuple(jnp.moveaxis(t, 1, 0) for t in (r, w, k, v, kk, a))
    S, ys = lax.scan(step, S0.astype(F32), xs)
    return jnp.moveaxis(ys, 0, 1), S


def rwkv7_mix(ps, gate, v_first, S0, w0, w2, a0, a2, k_k, k_a, r_k, gn_w, gn_b, vres):
    Bsz, T, _ = ps.shape
    r, k, v, wl, al = jnp.split(
        ps, [RWKV_WIDTH, 2 * RWKV_WIDTH, 3 * RWKV_WIDTH, 3 * RWKV_WIDTH + LORA_W], axis=-1)
    wlog = -jax.nn.softplus(-(w0.astype(F32) + jnp.tanh(wl) @ w2.astype(F32))) - 0.5
    decay = jnp.exp(-jnp.exp(wlog))
    a = jax.nn.sigmoid(a0.astype(F32) + al @ a2.astype(F32))
    if vres is None:
        v_first = v
    else:
        v0, v1, v2 = vres
        v = v + (v_first - v) * jax.nn.sigmoid(
            v0.astype(F32) + (v @ v1.astype(F32)) @ v2.astype(F32))
    heads = lambda t: t.reshape(Bsz, T, RWKV_HEADS, RWKV_HEAD)
    kk = heads(k * k_k.astype(F32))
    kk = kk * lax.rsqrt(jnp.maximum(jnp.sum(kk * kk, axis=-1, keepdims=True), 1e-24))
    k = k * (1.0 + (a - 1.0) * k_a.astype(F32))
    rh, kh, vh, ah = heads(r), heads(k), heads(v), heads(a)
    y, S = wkv_scan(rh, heads(decay), kh, vh, kk, ah, S0)
    mu = jnp.mean(y, axis=-1, keepdims=True)
    var = jnp.mean(jnp.square(y - mu), axis=-1, keepdims=True)
    y = (y - mu) * lax.rsqrt(var + GN_EPS)
    y = (y * gn_w.astype(F32).reshape(RWKV_HEADS, RWKV_HEAD)
         + gn_b.astype(F32).reshape(RWKV_HEADS, RWKV_HEAD))
    y = y + jnp.sum(rh * kh * r_k.astype(F32), axis=-1, keepdims=True) * vh
    out = y.reshape(Bsz, T, RWKV_WIDTH) * jax.nn.silu(gate.astype(F32))
    return out, v_first, S


def cmul(ar, ai, br, bi):
    return ar * br - ai * bi, ar * bi + ai * br


def s5_mix(u, gate, h0_re, h0_im, lam_re, lam_im, log_dt, b_re, b_im, c_re, c_im, d_skip, w_glu):
    Bsz, T, _ = u.shape
    uf = u.astype(F32)
    ug = uf.reshape(Bsz, T, S5_GROUPS, S5_GROUP)
    dt = jnp.exp(log_dt.astype(F32))[:, None]
    lr, li = lam_re.astype(F32), lam_im.astype(F32)
    mag = jnp.exp(lr * dt)
    lb_re, lb_im = mag * jnp.cos(li * dt), mag * jnp.sin(li * dt)
    q_re, q_im = lb_re - 1.0, lb_im
    den = lr * lr + li * li
    f_re = (q_re * lr + q_im * li) / den
    f_im = (q_im * lr - q_re * li) / den
    bb_re, bb_im = cmul(f_re[..., None], f_im[..., None], b_re.astype(F32), b_im.astype(F32))
    bu_re = jnp.einsum('gnc,btgc->btgn', bb_re, ug)
    bu_im = jnp.einsum('gnc,btgc->btgn', bb_im, ug)
    a_re = jnp.broadcast_to(lb_re, bu_re.shape)
    a_im = jnp.broadcast_to(lb_im, bu_im.shape)

    def combine(e1, e2):
        a1r, a1i, b1r, b1i = e1
        a2r, a2i, b2r, b2i = e2
        ar, ai = cmul(a1r, a1i, a2r, a2i)
        br, bi = cmul(a2r, a2i, b1r, b1i)
        return ar, ai, br + b2r, bi + b2i

    Ar, Ai, Hr, Hi = lax.associative_scan(combine, (a_re, a_im, bu_re, bu_im), axis=1)
    cr, ci = cmul(Ar, Ai, h0_re.astype(F32)[:, None], h0_im.astype(F32)[:, None])
    Hr = Hr + cr
    Hi = Hi + ci
    y = (jnp.einsum('gcn,btgn->btgc', c_re.astype(F32), Hr)
         - jnp.einsum('gcn,btgn->btgc', c_im.astype(F32), Hi))
    y = y.reshape(Bsz, T, S5_WIDTH) + d_skip.astype(F32) * uf
    y = jax.nn.gelu(y, approximate=False)
    y = y * jax.nn.sigmoid(y @ w_glu.astype(F32))
    out = y * jax.nn.silu(gate.astype(F32))
    return out, Hr[:, -1], Hi[:, -1]


def cross_attend(xn, mk, mv, wq, wo):
    Bsz, T, _ = xn.shape
    q = (xn @ wq).reshape(Bsz, T, X_HEADS, X_HEAD_DIM)
    s = jnp.einsum('bthd,bmhd->bhtm', q, mk).astype(F32) / math.sqrt(X_HEAD_DIM)
    p = jax.nn.softmax(s, axis=-1).astype(mv.dtype)
    o = jnp.einsum('bhtm,bmhd->bthd', p, mv).reshape(Bsz, T, D_MODEL)
    return o @ wo


def run_trunk(x, shift0, wkv0, s5re0, s5im0, mem_k, mem_v, P):
    new_shift, new_wkv, new_re, new_im = [], [], [], []
    v_first = None
    for l in range(DEPTH):
        xn = rms_norm(x, P['norm_mix'][l])
        proj = xn @ P['w_in'][l]
        p_sh = proj[..., :SHIFT_COLS]
        new_shift.append(p_sh[:, -1])
        ps = token_shift(p_sh, shift0[l], P['mu_shift'][l]).astype(F32)
        g_rwkv = proj[..., SHIFT_COLS:SHIFT_COLS + RWKV_WIDTH]
        u_s5 = proj[..., SHIFT_COLS + RWKV_WIDTH:SHIFT_COLS + RWKV_WIDTH + S5_WIDTH]
        g_s5 = proj[..., SHIFT_COLS + RWKV_WIDTH + S5_WIDTH:]
        vres = None if l == 0 else (P['v0'][l - 1], P['v1'][l - 1], P['v2'][l - 1])
        o_rwkv, v_first, S = rwkv7_mix(
            ps, g_rwkv, v_first, wkv0[l], P['w0'][l], P['w2'][l], P['a0'][l], P['a2'][l],
            P['k_k'][l], P['k_a'][l], P['r_k'][l], P['gn_w'][l], P['gn_b'][l], vres)
        o_s5, hr, hi = s5_mix(
            u_s5, g_s5, s5re0[l], s5im0[l], P['lam_re'][l], P['lam_im'][l], P['log_dt'][l],
            P['b_re'][l], P['b_im'][l], P['c_re'][l], P['c_im'][l], P['d_skip'][l], P['w_glu'][l])
        new_wkv.append(S)
        new_re.append(hr)
        new_im.append(hi)
        mix = jnp.concatenate([o_rwkv, o_s5], axis=-1).astype(x.dtype)
        x = x + mix @ P['w_out'][l]
        xc = rms_norm(x, P['norm_x'][l])
        x = x + cross_attend(xc, mem_k[l], mem_v[l], P['wq'][l], P['wo'][l])
    y = rms_norm(x, P['norm_f'])
    return y, jnp.stack(new_shift), jnp.stack(new_wkv), jnp.stack(new_re), jnp.stack(new_im)


def setup_inputs(seed: int = 0) -> dict:
    key = jax.random.key(seed)
    ks = iter(jax.random.split(key, 64))
    nrm = lambda shape, scale: jax.random.normal(next(ks), shape, F32) * scale
    L, D, R, G, N, C = DEPTH, D_MODEL, RWKV_WIDTH, S5_GROUPS, S5_STATE, S5_GROUP
    return {
        'x_prompt': nrm((BATCH, SEQ, D), 1.0),
        'x_sample': nrm((DEC_BATCH, DEC_SEQ, D), 1.0),
        'state_shift': nrm((L, DEC_BATCH, SHIFT_COLS), 1.0),
        'state_wkv': nrm((L, DEC_BATCH, RWKV_HEADS, RWKV_HEAD, RWKV_HEAD), 0.5),
        'state_s5_re': nrm((L, DEC_BATCH, G, N), 1.0),
        'state_s5_im': nrm((L, DEC_BATCH, G, N), 1.0),
        'cache_mem_k': nrm((L, DEC_BATCH, MEM_LEN, X_HEADS, X_HEAD_DIM), 1.0),
        'cache_mem_v': nrm((L, DEC_BATCH, MEM_LEN, X_HEADS, X_HEAD_DIM), 1.0),
        'mem_prompt': nrm((BATCH, MEM_LEN, D), 1.0),
        'norm_mix': 1.0 + nrm((L, D), 0.02),
        'w_in': nrm((L, D, IN_COLS), D ** -0.5),
        'mu_shift': jax.random.uniform(next(ks), (L, SHIFT_COLS), F32),
        'w0': nrm((L, R), 0.5),
        'w2': nrm((L, LORA_W, R), 0.1 * LORA_W ** -0.5),
        'a0': nrm((L, R), 0.1),
        'a2': nrm((L, LORA_A, R), 0.1 * LORA_A ** -0.5),
        'v0': nrm((L - 1, R), 0.1),
        'v1': nrm((L - 1, R, LORA_V), R ** -0.5),
        'v2': nrm((L - 1, LORA_V, R), 0.1 * LORA_V ** -0.5),
        'k_k': 0.85 + nrm((L, R), 0.02),
        'k_a': 1.0 + nrm((L, R), 0.02),
        'r_k': nrm((L, RWKV_HEADS, RWKV_HEAD), 0.1),
        'gn_w': 1.0 + nrm((L, R), 0.02),
        'gn_b': nrm((L, R), 0.02),
        'lam_re': -0.5 + nrm((L, G, N), 0.01),
        'lam_im': jnp.pi * jnp.arange(N, dtype=F32)[None, None, :] + nrm((L, G, N), 0.01),
        'log_dt': jax.random.uniform(next(ks), (L, G), F32, math.log(0.001), math.log(0.1)),
        'b_re': nrm((L, G, N, C), (2 * C) ** -0.5),
        'b_im': nrm((L, G, N, C), (2 * C) ** -0.5),
        'c_re': nrm((L, G, C, N), (2 * N) ** -0.5),
        'c_im': nrm((L, G, C, N), (2 * N) ** -0.5),
        'd_skip': nrm((L, S5_WIDTH), 1.0),
        'w_glu': nrm((L, S5_WIDTH, S5_WIDTH), S5_WIDTH ** -0.5),
        'w_out': nrm((L, MIX_WIDTH, D), MIX_WIDTH ** -0.5),
        'norm_x': 1.0 + nrm((L, D), 0.02),
        'norm_mem': 1.0 + nrm((L, D), 0.02),
        'wq': nrm((L, D, D), D ** -0.5),
        'wk': nrm((L, D, D), D ** -0.5),
        'wv': nrm((L, D, D), D ** -0.5),
        'wo': nrm((L, D, D), D ** -0.5),
        'norm_f': 1.0 + nrm((D,), 0.02),
    }


def reference(x_prompt, x_sample, state_shift, state_wkv, state_s5_re, state_s5_im,
              cache_mem_k, cache_mem_v, mem_prompt, norm_mix, w_in, mu_shift, w0, w2, a0, a2,
              v0, v1, v2, k_k, k_a, r_k, gn_w, gn_b, lam_re, lam_im, log_dt, b_re, b_im,
              c_re, c_im, d_skip, w_glu, w_out, norm_x, norm_mem, wq, wk, wv, wo, norm_f):
    P = dict(norm_mix=norm_mix, w_in=w_in, mu_shift=mu_shift, w0=w0, w2=w2, a0=a0, a2=a2,
             v0=v0, v1=v1, v2=v2, k_k=k_k, k_a=k_a, r_k=r_k, gn_w=gn_w, gn_b=gn_b,
             lam_re=lam_re, lam_im=lam_im, log_dt=log_dt, b_re=b_re, b_im=b_im,
             c_re=c_re, c_im=c_im, d_skip=d_skip, w_glu=w_glu, w_out=w_out,
             norm_x=norm_x, wq=wq, wo=wo, norm_f=norm_f)
    Bp, Mp = mem_prompt.shape[0], mem_prompt.shape[1]
    mks, mvs = [], []
    for l in range(DEPTH):
        mn = rms_norm(mem_prompt, norm_mem[l])
        mks.append((mn @ wk[l]).reshape(Bp, Mp, X_HEADS, X_HEAD_DIM))
        mvs.append((mn @ wv[l]).reshape(Bp, Mp, X_HEADS, X_HEAD_DIM))
    p_mem_k = jnp.stack(mks)
    p_mem_v = jnp.stack(mvs)
    z_shift = jnp.zeros((DEPTH, Bp, SHIFT_COLS), x_prompt.dtype)
    z_wkv = jnp.zeros((DEPTH, Bp, RWKV_HEADS, RWKV_HEAD, RWKV_HEAD), F32)
    z_s5 = jnp.zeros((DEPTH, Bp, S5_GROUPS, S5_STATE), F32)
    y_prompt, p_shift, p_wkv, p_s5_re, p_s5_im = run_trunk(
        x_prompt, z_shift, z_wkv, z_s5, z_s5, p_mem_k, p_mem_v, P)
    y_sample, s_shift, s_wkv, s_s5_re, s_s5_im = run_trunk(
        x_sample, state_shift, state_wkv, state_s5_re, state_s5_im, cache_mem_k, cache_mem_v, P)
    return (y_prompt, y_sample, p_shift, p_wkv, p_s5_re, p_s5_im, p_mem_k, p_mem_v,
            s_shift, s_wkv, s_s5_re, s_s5_im)
```

```python
import math
import numpy as np
import concourse.bass as bass
import concourse.mybir as mybir
from concourse.bass_utils import run_bass_kernel_spmd

F32 = mybir.dt.float32
BF16 = mybir.dt.bfloat16
I32 = mybir.dt.int32
AF = mybir.ActivationFunctionType
ALU = mybir.AluOpType

D = 1024
DEPTH = 4
SEQ = 2048
NB_S = 16
T_S = 4
RW = 512
SHIFT = 1664
INC = 3200
MEM = 256
C0 = math.exp(-0.5)
NORM_EPS = 1e-6
GN_EPS = 64e-5
TWO_PI = 2.0 * math.pi
CW1 = 6.28125
CW2 = TWO_PI - CW1


class Buf:
    __slots__ = ("w", "r", "sem", "semv", "excl")

    def __init__(self, excl=False):
        self.w = None
        self.r = {}
        self.sem = None
        self.semv = 0
        self.excl = excl


class Prog:
    ENG = ("pe", "act", "dve", "pool", "sp")

    def __init__(self, nc):
        self.nc = nc
        self.ops = {e: [] for e in self.ENG}
        self.cnt = {e: 0 for e in self.ENG}
        self.sems = {}
        self.seen = {e: {} for e in self.ENG}
        self.nsem = 0
        for e in self.ENG:
            self.sems[("eng", e)] = nc.alloc_semaphore(name="prog_" + e)
        self.out_events = {}
        self.ctx = []
        self.ctxb = []
        self.sb_bytes = 0
        self.dmav = {}
        self.pending = {e: {} for e in self.ENG}

    def sb(self, name, shape, dtype=F32):
        g = self.nc.sbuf_tensor(name, list(shape), dtype)
        t = g.__enter__()
        self.ctx.append(g)
        n = 1
        for s in shape[1:]:
            n *= s
        nb = n * (4 if dtype in (F32, I32) else 2)
        self.sb_bytes += nb
        self.ctxb.append(nb)
        return t

    def mark(self):
        return len(self.ctx)

    def release(self, mark):
        while len(self.ctx) > mark:
            self.ctx.pop().__exit__(None, None, None)
            self.sb_bytes -= self.ctxb.pop()

    def barrier(self):
        cur = {("eng", e): self.cnt[e] for e in self.ENG}
        cur.update(self.dmav)
        for e in self.ENG:
            for k, v in cur.items():
                if v > 0 and self.pending[e].get(k, 0) < v:
                    self.pending[e][k] = v

    def ps(self, name, shape, dtype=F32):
        g = self.nc.psum_tensor(name, list(shape), dtype)
        t = g.__enter__()
        self.ctx.append(g)
        self.ctxb.append(0)
        return t

    def _dsem(self, b):
        if b.sem is None:
            self.nsem += 1
            key = ("dma", self.nsem)
            self.sems[key] = self.nc.alloc_semaphore(name="dq%d" % self.nsem)
            b.sem = key
        return b.sem

    def pe_fence(self):
        if self.cnt["pe"] > 0:
            self.pending["pe"][("eng", "pe")] = self.cnt["pe"]

    def _waits(self, eng, reads, writes):
        need = {}

        def add(ev):
            if ev is None:
                return
            k, v = ev
            if need.get(k, 0) < v:
                need[k] = v
        for b in reads:
            add(b.w)
        for b in writes:
            add(b.w)
            for k, v in b.r.items():
                add((k, v))
        own = ("eng", eng)
        if eng == "pe":
            need.pop(own, None)
        if self.pending[eng]:
            for k, v in self.pending[eng].items():
                add((k, v))
            self.pending[eng] = {}
        out = []
        seen = self.seen[eng]
        for k, v in need.items():
            if seen.get(k, 0) < v:
                seen[k] = v
                out.append((k, v))
        return out

    def _commit(self, ev, reads, writes):
        k, v = ev
        for b in reads:
            if b.r.get(k, 0) < v:
                b.r[k] = v
        for b in writes:
            b.w = ev
            b.r = {}

    def op(self, eng, fn, reads=(), writes=()):
        if any(b.excl for b in reads):
            writes = list(writes) + [b for b in reads if b.excl]
            reads = [b for b in reads if not b.excl]
        waits = self._waits(eng, reads, writes)
        self.cnt[eng] += 1
        ev = (("eng", eng), self.cnt[eng])
        self.ops[eng].append((waits, fn, (ev[0], 1)))
        self._commit(ev, reads, writes)
        return ev

    def dma(self, eng, out, in_, reads=(), writes=(), sembuf=None, final=False, **kw):
        if sembuf is None:
            sembuf = writes[0] if writes else reads[0]
        waits = self._waits(eng, reads, writes)
        key = self._dsem(sembuf)
        sembuf.semv += 16
        ev = (key, sembuf.semv)
        self.dmav[key] = sembuf.semv

        def fn(e, out=out, in_=in_, kw=kw):
            return e.dma_start(out=out, in_=in_, **kw)
        self.ops[eng].append((waits, fn, (key, 16)))
        self._commit(ev, reads, writes)
        if final and self.out_events.get(key, 0) < ev[1]:
            self.out_events[key] = ev[1]
        return ev

    def emit(self, final=True):
        nc = self.nc
        fin = list(self.out_events.items()) if final else []
        hmap = {"pe": "tensor", "act": "scalar", "dve": "vector", "pool": "gpsimd", "sp": "sync"}
        with nc.Block() as block:
            for eng in self.ENG:
                ops = self.ops[eng]
                extra = fin if eng == "sp" else []

                def body(e, ops=ops, extra=extra):
                    for waits, fn, inc in ops:
                        for k, v in waits:
                            e.wait_ge(self.sems[k], v)
                        ins = fn(e)
                        ins.then_inc(self.sems[inc[0]], inc[1])
                    for k, v in extra:
                        e.wait_ge(self.sems[k], v)
                getattr(block, hmap[eng])(body)
        self.ops = {e: [] for e in self.ENG}
        if final:
            self.release(0)


class Rot:
    def __init__(self, P, name, n, shape, dtype=F32, psum=False):
        self.t = [(P.ps if psum else P.sb)("%s%d" % (name, i), shape, dtype) for i in range(n)]
        self.b = [Buf() for _ in range(n)]
        self.i = 0

    def next(self):
        i = self.i
        self.i = (i + 1) % len(self.t)
        return self.t[i], self.b[i]


class K:
    def __init__(self, cfg):
        self.cfg = cfg
        self.TN = cfg.get("TN", 256)
        self.NT = cfg.get("NT", SEQ // self.TN)
        self.NL = cfg.get("NL", DEPTH)
        self.sample = cfg.get("sample", True)
        self.nc = bass.Bass("TRN2", target_bir_lowering=False)
        self.P = Prog(self.nc)
        self.dram = {}

    def din(self, name, shape, dt=F32):
        a = self.nc.dram_tensor(name, list(shape), dt, kind="ExternalInput").ap()
        self.dram[name] = a
        return a

    def dout(self, name, shape, dt=F32):
        a = self.nc.dram_tensor(name, list(shape), dt, kind="ExternalOutput").ap()
        self.dram[name] = a
        return a

    def dscr(self, name, shape, dt=F32):
        a = self.nc.dram_tensor(name, list(shape), dt, kind="Internal").ap()
        self.dram[name] = a
        return a

    def mm(self, out, lhsT, rhs, start, stop, r, w):
        return self.P.op("pe", lambda e: e.matmul(out, lhsT=lhsT, rhs=rhs, start=start, stop=stop), reads=r, writes=w)

    def tr(self, out, in_, ident, r, w):
        return self.P.op("pe", lambda e: e.transpose(out=out, in_=in_, identity=ident), reads=r, writes=w)

    def act(self, out, in_, func, r, w, bias=None, scale=None, eng="act"):
        kw = {}
        if bias is not None:
            kw["bias"] = bias
        if scale is not None:
            kw["scale"] = scale
        return self.P.op(eng, lambda e: e.activation(out=out, in_=in_, func=func, **kw), reads=r, writes=w)

    def tt(self, out, in0, in1, op, r, w, eng="dve"):
        return self.P.op(eng, lambda e: e.tensor_tensor(out=out, in0=in0, in1=in1, op=op), reads=r, writes=w)

    def ts(self, out, in0, s1, op0, r, w, s2=None, op1=None, eng="dve"):
        if op1 is None:
            return self.P.op(eng, lambda e: e.tensor_scalar(out=out, in0=in0, scalar1=s1, scalar2=None, op0=op0), reads=r, writes=w)
        return self.P.op(eng, lambda e: e.tensor_scalar(out=out, in0=in0, scalar1=s1, scalar2=s2, op0=op0, op1=op1), reads=r, writes=w)

    def stt(self, out, in0, scalar, in1, op0, op1, r, w):
        return self.P.op("dve", lambda e: e.scalar_tensor_tensor(out=out, in0=in0, scalar=scalar, in1=in1, op0=op0, op1=op1), reads=r, writes=w)

    def cp(self, out, in_, r, w, eng="dve"):
        if eng == "act":
            return self.act(out, in_, AF.Copy, r, w)
        return self.P.op(eng, lambda e: e.tensor_copy(out=out, in_=in_), reads=r, writes=w)

    def recip(self, out, in_, r, w):
        return self.P.op("dve", lambda e: e.reciprocal(out=out, in_=in_), reads=r, writes=w)

    def recipf(self, out, in_, r, w):
        return self.P.op("dve", lambda e: e.reciprocal_approx_fast(out=out, in_=in_), reads=r, writes=w)

    def scan(self, out, d0, d1, init, r, w):
        return self.P.op("dve", lambda e: e.tensor_tensor_scan(out=out, data0=d0, data1=d1, initial=init, op0=ALU.mult, op1=ALU.add), reads=r, writes=w)

    def memset(self, ap, val, w, eng="pool"):
        return self.P.op(eng, lambda e: e.memset(ap, val), writes=w)

    def build(self):
        P, nc = self.P, self.nc
        TN, NT, NL = self.TN, self.NT, self.NL
        SQ = NT * TN
        xp = self.din("xp", [SQ, D])
        memp = self.din("memp", [MEM, D])
        win = self.din("w_in", [DEPTH, D, INC])
        wout = self.din("w_out", [DEPTH, D, D])
        wq = self.din("wq", [DEPTH, D, D])
        wk = self.din("wk", [DEPTH, D, D])
        wv = self.din("wv", [DEPTH, D, D])
        wo = self.din("wo", [DEPTH, D, D])
        wglu = self.din("w_glu", [DEPTH, RW, RW])
        w2 = self.din("w2", [DEPTH, 64, RW])
        a2 = self.din("a2", [DEPTH, 64, RW])
        v1 = self.din("v1", [DEPTH - 1, RW, 32])
        v2 = self.din("v2", [DEPTH - 1, 32, RW])
        prm = {}
        for nm, wd, L in (("norm_mix", D, DEPTH), ("norm_x", D, DEPTH), ("norm_mem", D, DEPTH), ("norm_f", D, 1),
                          ("mu_shift", SHIFT, DEPTH), ("w0", RW, DEPTH), ("a0", RW, DEPTH), ("k_k", RW, DEPTH),
                          ("k_a", RW, DEPTH), ("gn_w", RW, DEPTH), ("gn_b", RW, DEPTH), ("r_k", RW, DEPTH),
                          ("d_skip", RW, DEPTH), ("v0", RW, DEPTH - 1), ("lam_re", 2048, DEPTH), ("lam_im", 2048, DEPTH)):
            prm[nm] = (self.din(nm, [L, wd]), wd // 128, L)
        logdt = self.din("log_dt", [DEPTH, 32])
        bre_d = self.din("b_re", [DEPTH, 32, 64, 16])
        bim_d = self.din("b_im", [DEPTH, 32, 64, 16])
        cre_d = self.din("c_re", [DEPTH, 32, 16, 64])
        cim_d = self.din("c_im", [DEPTH, 32, 16, 64])

        yp = self.dout("y_p", [SQ, D])
        o_pshift = self.dout("p_shift", [DEPTH, SHIFT])
        o_pwkv = self.dout("p_wkv", [DEPTH, 8, 64, 64])
        o_ps5re = self.dout("p_s5_re", [DEPTH, 32, 64])
        o_ps5im = self.dout("p_s5_im", [DEPTH, 32, 64])
        o_pmk = self.dout("p_mem_k", [DEPTH, MEM, D])
        o_pmv = self.dout("p_mem_v", [DEPTH, MEM, D])

        s_win = self.dscr("s_win", [DEPTH, 5, 128, 8 * 640], BF16)
        s_wout = self.dscr("s_wout", [DEPTH, 2, 128, 8 * 512], BF16)
        s_wq = self.dscr("s_wq", [DEPTH, 2, 128, 8 * 512], BF16)
        s_wo = self.dscr("s_wo", [DEPTH, 2, 128, 8 * 512], BF16)
        s_kt = self.dscr("s_kt", [DEPTH, 128, 8 * MEM], BF16)
        s_vm = self.dscr("s_vm", [DEPTH, 128, 2 * D], BF16)
        s_bc = self.dscr("s_bc", [DEPTH, 128, 4, 4 * 4 * 128], BF16)
        s_cs = self.dscr("s_cs", [DEPTH, 128, 2 * 16 * 129], F32)
        b_swin = [Buf() for _ in range(DEPTH)]
        b_swout = [Buf() for _ in range(DEPTH)]
        b_swq = [Buf() for _ in range(DEPTH)]
        b_swo = [Buf() for _ in range(DEPTH)]
        b_skt = [Buf() for _ in range(DEPTH)]
        b_svm = [Buf() for _ in range(DEPTH)]
        b_sbc = [Buf() for _ in range(DEPTH)]
        b_scs = [Buf() for _ in range(DEPTH)]

        PS = [P.ps("psb%d" % i, [128, 512]) for i in range(8)]
        BPS = [Buf(excl=True) for _ in range(8)]
        self.pd_i = 0

        self.pd_banks = [0, 1]

        def pdense():
            bk = self.pd_banks
            self.pd_i = (self.pd_i + 1) % len(bk)
            i = bk[self.pd_i]
            return PS[i], BPS[i]

        identf = P.sb("identf", [128, 128])
        identb = P.sb("identb", [128, 128], BF16)
        onesf = P.sb("onesf", [128, 128])
        bonesf = P.sb("bonesf", [128, 128])
        bonesb = P.sb("bonesb", [128, 128], BF16)
        onesb = P.sb("onesb", [128, 128], BF16)
        mska = P.sb("mska", [128, 128])
        mskl = P.sb("mskl", [128, 64])
        cmask = P.sb("cmask", [128, TN])
        tau = P.sb("tau", [128, 129])
        bc = Buf()
        self.memset(identf[:], 0.0, [bc])
        P.op("pool", lambda e: e.affine_select(out=identf[:], in_=identf[:], pattern=[[-1, 128]], compare_op=ALU.not_equal, fill=1.0, base=0, channel_multiplier=1), reads=[bc], writes=[bc])
        self.cp(identb[:], identf[:], [bc], [bc], eng="pool")
        self.memset(onesf[:], 1.0, [bc])
        self.memset(onesb[:], 1.0, [bc])
        self.memset(bonesf[:], 0.0, [bc])
        self.memset(bonesf[0:64, 0:64], 1.0 / 64.0, [bc])
        self.memset(bonesf[64:128, 64:128], 1.0 / 64.0, [bc])
        self.memset(bonesb[:], 0.0, [bc])
        self.memset(bonesb[0:64, 0:64], 1.0, [bc])
        self.memset(bonesb[64:128, 64:128], 1.0, [bc])
        self.memset(mska[:], 1.0, [bc])
        for hb in (0, 64):
            P.op("pool", lambda e, hb=hb: e.affine_select(out=mska[hb:hb + 64, 0:64], in_=mska[hb:hb + 64, 0:64], pattern=[[1, 64]], compare_op=ALU.is_gt, fill=0.0, base=0, channel_multiplier=-1), reads=[bc], writes=[bc])
            P.op("pool", lambda e, hb=hb: e.affine_select(out=mska[hb:hb + 64, 64:128], in_=mska[hb:hb + 64, 64:128], pattern=[[1, 64]], compare_op=ALU.is_ge, fill=0.0, base=0, channel_multiplier=-1), reads=[bc], writes=[bc])
        self.memset(mskl[:], 1.0, [bc])
        for hb in (0, 64):
            P.op("pool", lambda e, hb=hb: e.affine_select(out=mskl[hb:hb + 64, :], in_=mskl[hb:hb + 64, :], pattern=[[-1, 64]], compare_op=ALU.is_gt, fill=0.0, base=0, channel_multiplier=1), reads=[bc], writes=[bc])
        self.memset(cmask[:], 1.0, [bc])
        self.memset(cmask[:].rearrange("p (a b) -> p a b", b=64)[:, :, 0:1], 0.0, [bc])
        P.op("pool", lambda e: e.iota(tau[:], pattern=[[1, 129]], base=0, channel_multiplier=0, allow_small_or_imprecise_dtypes=True), writes=[bc])
        self.identf, self.identb, self.bc = identf, identb, bc

        if self.cfg.get("stop") == "const":
            dbg = self.dout("dbg", [128, 128])
            P.dma("sp", dbg[:, :], mska[:], reads=[bc], final=True)
            P.emit()
            return nc
        order = [["norm_mix", "norm_x", "norm_mem", "norm_f"],
                 ["mu_shift", "w0", "a0", "k_k", "k_a"],
                 ["gn_w", "gn_b", "r_k", "d_skip", "v0"],
                 ["lam_re", "lam_im"]]
        ncols = sum(prm[n][1] * prm[n][2] for g in order for n in g)
        PRM = P.sb("PRM", [128, ncols + 64])
        bprm = Buf()
        col = {}
        c0 = 0
        for gi, g in enumerate(order):
            rows = sum(prm[n][1] * prm[n][2] for n in g)
            stg = P.sb("stg%d" % gi, [128, 128])
            bst = Buf()
            r0 = 0
            for n in g:
                ap, nch, L = prm[n]
                nr = nch * L
                P.dma("sp", stg[r0:r0 + nr, :], ap.rearrange("l (c p) -> (l c) p", p=128), writes=[bst])
                col[n] = (c0 + r0, nch)
                r0 += nr
            pt, bpt = PS[2 + gi % 2], BPS[2 + gi % 2]
            self.tr(pt[:, 0:rows], stg[0:rows, :], identf[0:rows, 0:rows], [bst, bc], [bpt])
            self.cp(PRM[:, c0:c0 + rows], pt[:, 0:rows], [bpt], [bprm], eng="act")
            c0 += rows
        self.PRM, self.bprm, self.col = PRM, bprm, col

        def pc(name, l, c):
            c00, nch = col[name]
            return PRM[:, c00 + l * nch + c: c00 + l * nch + c + 1]
        self.pc = pc
        OMM = P.sb("OMM", [128, DEPTH * 13])
        OMK = P.sb("OMK", [128, DEPTH * 4])
        cm, _ = col["mu_shift"]
        ck, _ = col["k_a"]
        self.ts(OMM[:], PRM[:, cm:cm + DEPTH * 13], -1.0, ALU.mult, [bprm], [bprm], s2=1.0, op1=ALU.add)
        self.ts(OMK[:], PRM[:, ck:ck + DEPTH * 4], -1.0, ALU.mult, [bprm], [bprm], s2=1.0, op1=ALU.add)

        if self.cfg.get("stop") == "prm":
            dbg = self.dout("dbg", [128, ncols])
            P.dma("sp", dbg[:, :], PRM[:, 0:ncols], reads=[bprm], final=True)
            P.emit()
            return nc
        def precast(l):
            for h in range(5):
                P.dma("pool", s_win[l, h].rearrange("p (kc c) -> p kc c", kc=8), win[l, :, h * 640:(h + 1) * 640].rearrange("(kc p) c -> p kc c", p=128), writes=[b_swin[l]])
            for (src, dst, bb) in ((wout, s_wout, b_swout), (wq, s_wq, b_swq), (wo, s_wo, b_swo)):
                for h in range(2):
                    P.dma("pool", dst[l, h].rearrange("p (kc c) -> p kc c", kc=8), src[l, :, h * 512:(h + 1) * 512].rearrange("(kc p) c -> p kc c", p=128), writes=[bb[l]])
        if self.cfg.get("stop") == "pre":
            precast(0)

        if self.cfg.get("stop") == "pre":
            dbgt = P.sb("dbgt", [128, 8, 640], BF16)
            bdbg = Buf()
            P.dma("sp", dbgt[:], s_win[0, 0].rearrange("p (kc c) -> p kc c", kc=8), reads=[b_swin[0]], writes=[bdbg])
            dbg = self.dout("dbg", [128, 8, 640], BF16)
            P.dma("sp", dbg[:, :, :], dbgt[:], reads=[bdbg], final=True)
            P.emit()
            return nc
        WB = Rot(P, "WB", 2, [128, 8, 640], BF16)

        def wload(src_l, c_lo, ncol, rbuf, eng="sp"):
            t, b = WB.next()
            P.dma(eng, t[:, :, 0:ncol], src_l[:, c_lo:c_lo + ncol].rearrange("(kc p) c -> p kc c", p=128), reads=[rbuf] if rbuf else [], writes=[b])
            return t, b

        def wload_t(src_lg, ncol, rbuf):
            t, b = WB.next()
            P.dma("sp", t[:, :, 0:ncol], src_lg.rearrange("p (kc c) -> p kc c", kc=8), reads=[rbuf], writes=[b])
            return t, b

        xT = P.sb("xT", [128, 8, TN])
        bxT = Buf()
        ACTB = P.sb("ACTB", [128, 8, TN], BF16)
        bACT = Buf()
        SCRB = P.sb("SCRB", [128, 8, TN], BF16)
        bSCR = Buf()
        rstd = P.sb("rstd", [128, TN])
        brstd = Buf()

        def rmsnorm(gname, l, N):
            self.act(SCRB[:, :, 0:N], xT[:, :, 0:N], AF.Square, [bxT], [bSCR])
            pt, bpt = pdense()
            for kc in range(8):
                self.mm(pt[:, 0:N], onesb[:], SCRB[:, kc, 0:N], kc == 0, kc == 7, [bSCR, bc], [bpt])
            self.act(rstd[:, 0:N], pt[:, 0:N], AF.Ln, [bpt], [brstd], bias=NORM_EPS, scale=1.0 / D)
            self.act(rstd[:, 0:N], rstd[:, 0:N], AF.Exp, [brstd], [brstd], scale=-0.5)
            for kc in range(8):
                self.stt(ACTB[:, kc, 0:N], xT[:, kc, 0:N], pc(gname, l, kc), rstd[:, 0:N], ALU.mult, ALU.mult, [bxT, brstd, bprm], [bACT])

        S5P = P.sb("S5P", [128, 12, DEPTH * 16])
        setup_mark = P.mark()
        mtm = P.sb("mtm", [128, 2, D])
        bmtm = Buf()
        P.dma("sp", mtm[:], memp.rearrange("(a p) d -> p a d", p=128), writes=[bmtm])
        mss = P.sb("mss", [128, 2])
        bmss = Buf()
        mjunk = P.sb("mjunk", [128, D], BF16)
        for a in range(2):
            P.op("act", lambda e, a=a: e.activation(out=mjunk[:], in_=mtm[:, a, :], func=AF.Square, accum_out=mss[:, a:a + 1]), reads=[bmtm], writes=[bmss])
        self.act(mss[:], mss[:], AF.Sqrt, [bmss], [bmss], bias=NORM_EPS, scale=1.0 / D)
        self.recip(mss[:], mss[:], [bmss], [bmss])
        for a in range(2):
            self.ts(mtm[:, a, :], mtm[:, a, :], mss[:, a:a + 1], ALU.mult, [bmtm, bmss], [bmtm])
        mT = P.sb("mT", [128, 8, MEM])
        bmT = Buf()
        for a in range(2):
            for half in range(2):
                pt, bpt = PS[2 + half], BPS[2 + half]
                for q in range(4):
                    kc = half * 4 + q
                    self.tr(pt[:, q * 128:(q + 1) * 128], mtm[:, a, kc * 128:(kc + 1) * 128], identf[:], [bmtm, bc], [bpt])
                self.cp(mT[:, half * 4:(half + 1) * 4, a * 128:(a + 1) * 128], pt[:].rearrange("p (q m) -> p q m", q=4), [bpt], [bmT], eng="act")
        mnT = P.sb("mnT", [128, 8, MEM], BF16)
        bmnT = Buf()
        kts = P.sb("kts", [128, 8, MEM], BF16)
        bkts = Buf()
        vms = P.sb("vms", [128, 2, D], BF16)
        bvms = Buf()
        ostg = Rot(P, "ostg", 2, [128, 512])
        for l in range(NL):
            for kc in range(8):
                self.ts(mnT[:, kc, :], mT[:, kc, :], pc("norm_mem", l, kc), ALU.mult, [bmT, bprm], [bmnT])
            for which, wsrc in ((0, wk), (1, wv)):
                for half in range(2):
                    t, b = wload(wsrc[l], half * 512, 512, None, eng="pool")
                    if which == 0:
                        for cc in range(4):
                            pt, bpt = pdense()
                            for kc in range(8):
                                self.mm(pt[:, 0:MEM], t[:, kc, cc * 128:(cc + 1) * 128], mnT[:, kc, :], kc == 0, kc == 7, [b, bmnT], [bpt])
                            self.cp(kts[:, half * 4 + cc, :], pt[:, 0:MEM], [bpt], [bkts], eng="act")
                    for a in range(2):
                        pt, bpt = pdense()
                        for kc in range(8):
                            self.mm(pt[:, :], mnT[:, kc, a * 128:(a + 1) * 128], t[:, kc, 0:512], kc == 0, kc == 7, [b, bmnT], [bpt])
                        og, bog = ostg.next()
                        self.cp(og[:], pt[:], [bpt], [bog], eng="act")
                        if which == 1:
                            self.cp(vms[:, a, half * 512:(half + 1) * 512], pt[:], [bpt], [bvms], eng="dve")
                        dst = (o_pmk if which == 0 else o_pmv)[l, a * 128:(a + 1) * 128, half * 512:(half + 1) * 512]
                        P.dma("sp", dst, og[:], reads=[bog], final=True)
            P.dma("sp", s_kt[l], kts[:].rearrange("p a m -> p (a m)"), reads=[bkts], writes=[b_skt[l]])
            P.dma("sp", s_vm[l], vms[:].rearrange("p a m -> p (a m)"), reads=[bvms], writes=[b_svm[l]])

        precast(0)
        if self.cfg.get("stop") == "A":
            P.emit()
            return nc
        LD = P.sb("LD", [128, DEPTH, 16])
        bLD = Buf()
        for l in range(NL):
            for g2 in range(2):
                P.dma("sp", LD[g2 * 64:(g2 + 1) * 64, l, :], logdt[l:l + 1, g2::2].partition_broadcast(64), writes=[bLD], allow_slow_non_contiguous=True)
        clr, _ = col["lam_re"]
        cli, _ = col["lam_im"]
        NLK = NL * 16
        LR = PRM[:, clr:clr + NLK]
        LI = PRM[:, cli:cli + NLK]
        bS5P = Buf()
        DT, TH, RHO, LBR, LBI, FRE, FIM, T0, T1, T2, DEN, T3 = [S5P[:, i, 0:NLK] for i in range(12)]
        LDf = LD[:, 0:NL, :].rearrange("p l k -> p (l k)")
        r_, w_ = [bS5P, bprm, bLD], [bS5P]
        self.act(DT, LDf, AF.Exp, r_, w_)
        self.tt(TH, LI, DT, ALU.mult, r_, w_)
        self.tt(T0, LR, DT, ALU.mult, r_, w_)
        self.act(RHO, T0, AF.Exp, r_, w_)

        def sincos(out_sin, x, n, r, w, shift=0.0, tmpf=None, tmpi=None):
            self.ts(tmpf, x, 1.0, ALU.mult, r, w, s2=shift, op1=ALU.add)
            self.ts(tmpi, tmpf, 1.0 / TWO_PI, ALU.mult, r, w)
            self.cp(out_sin, tmpi, r, w)
            self.stt(tmpf, out_sin, -CW1, tmpf, ALU.mult, ALU.add, r, w)
            self.stt(tmpf, out_sin, -CW2, tmpf, ALU.mult, ALU.add, r, w)
            self.ts(tmpf, tmpf, math.pi, ALU.min, r, w, s2=-math.pi, op1=ALU.max)
            self.act(out_sin, tmpf, AF.Sin, r, w)
        S5I = P.sb("S5I", [128, 16 * 129], I32)
        S5F = P.sb("S5F", [128, 16 * 129])
        sincos(T1, TH, NLK, r_, w_, 0.0, T3, S5I[:, 0:NLK])
        sincos(T2, TH, NLK, r_, w_, math.pi / 2, T3, S5I[:, 0:NLK])
        self.tt(LBI, RHO, T1, ALU.mult, r_, w_)
        self.tt(LBR, RHO, T2, ALU.mult, r_, w_)
        self.ts(T0, LBR, -1.0, ALU.add, r_, w_)
        self.tt(T1, LR, LR, ALU.mult, r_, w_)
        self.tt(T2, LI, LI, ALU.mult, r_, w_)
        self.tt(DEN, T1, T2, ALU.add, r_, w_)
        self.recip(DEN, DEN, r_, w_)
        self.tt(T1, T0, LR, ALU.mult, r_, w_)
        self.tt(T2, LBI, LI, ALU.mult, r_, w_)
        self.tt(T1, T1, T2, ALU.add, r_, w_)
        self.tt(FRE, T1, DEN, ALU.mult, r_, w_)
        self.tt(T1, LBI, LR, ALU.mult, r_, w_)
        self.tt(T2, T0, LI, ALU.mult, r_, w_)
        self.tt(T1, T1, T2, ALU.subtract, r_, w_)
        self.tt(FIM, T1, DEN, ALU.mult, r_, w_)
        self.RHO, self.bS5P = RHO, bS5P
        self.LBR, self.LBI = LBR, LBI

        CS = P.sb("CS", [128, 2, 16, 129])
        bCS = Buf()
        ZB = P.sb("ZB", [128, 2, 16, 128])
        ZC = P.sb("ZC", [128, 2, 16, 128])
        ZT = P.sb("ZT", [128, 2, 16, 128])
        bZ = Buf()
        bZdB = [Buf() for _ in range(4)]
        bZdC = [Buf() for _ in range(4)]
        BCs = P.sb("BCs", [128, 4, 4, 4, 128], BF16)
        bBCs = Buf()
        for l in range(NL):
            thl = TH[:, l * 16:(l + 1) * 16]
            xx = S5F[:].rearrange("p (k t) -> p k t", k=16)
            self.tt(xx, tau[:].unsqueeze(1).to_broadcast([128, 16, 129]), thl.unsqueeze(2).to_broadcast([128, 16, 129]), ALU.mult, [bS5P, bc, bCS], [bCS])
            xflat = S5F[:]
            tmpf = P.sb("s5tmpf%d" % l, [128, 16 * 129]) if l == 0 else tmpf
            sincos(CS[:, 1].rearrange("p k t -> p (k t)"), xflat, 0, [bCS], [bCS], 0.0, tmpf[:], S5I[:])
            sincos(CS[:, 0].rearrange("p k t -> p (k t)"), xflat, 0, [bCS], [bCS], math.pi / 2, tmpf[:], S5I[:])
            P.dma("sp", s_cs[l], CS[:].rearrange("p a k t -> p (a k t)"), reads=[bCS], writes=[b_scs[l]])
            self.memset(ZB[:], 0.0, [bZ] + bZdB)
            self.memset(ZC[:], 0.0, [bZ] + bZdC, eng="dve")
            zi = 0
            for ri, (bsrc, csrc) in enumerate(((bre_d, cre_d), (bim_d, cim_d))):
                for g2 in range(2):
                    for q in range(4):
                        g8 = 2 * q + g2
                        src = bsrc[l].rearrange("(m e) n c -> e n m c", e=8)[g8]
                        P.dma("sp", ZB[g2 * 64:(g2 + 1) * 64, ri, q::4, 16 * g8:16 * g8 + 16], src, writes=[bZdB[zi % 4]], allow_slow_non_contiguous=True)
                        src = csrc[l].rearrange("(m e) c n -> e c m n", e=8)[g8]
                        P.dma("sp", ZC[16 * g8:16 * g8 + 16, ri, q::4, g2 * 64:(g2 + 1) * 64], src, writes=[bZdC[zi % 4]], allow_slow_non_contiguous=True)
                        zi += 1
            fre = FRE[:, l * 16:(l + 1) * 16].unsqueeze(2).to_broadcast([128, 16, 128])
            fim = FIM[:, l * 16:(l + 1) * 16].unsqueeze(2).to_broadcast([128, 16, 128])
            rz = [bZ, bS5P] + bZdB + bZdC
            self.tt(ZT[:, 0], ZB[:, 0], fre, ALU.mult, rz, [bZ])
            self.tt(ZT[:, 1], ZB[:, 1], fim, ALU.mult, rz, [bZ])
            self.tt(ZT[:, 0], ZT[:, 0], ZT[:, 1], ALU.subtract, rz, [bZ])
            self.tt(ZT[:, 1], ZB[:, 1], fre, ALU.mult, rz, [bZ])
            self.tt(ZB[:, 1], ZB[:, 0], fim, ALU.mult, rz, [bZ])
            self.tt(ZT[:, 1], ZT[:, 1], ZB[:, 1], ALU.add, rz, [bZ])
            for jc in range(4):
                for which in range(4):
                    pt, bpt = PS[2 + which % 2], BPS[2 + which % 2]
                    for kk in range(4):
                        k = jc * 4 + kk
                        src = (ZT[:, 0, k, :], ZT[:, 1, k, :], ZC[:, 0, k, :], ZC[:, 1, k, :])[which]
                        self.tr(pt[:, kk * 128:(kk + 1) * 128], src, identf[:], [bZ, bc] + bZdB + bZdC, [bpt])
                    dst = BCs[:, jc, :, which, :]
                    if which == 3:
                        self.act(dst, pt[:].rearrange("p (a b) -> p a b", a=4), AF.Copy, [bpt], [bBCs], scale=-1.0)
                    else:
                        self.act(dst, pt[:].rearrange("p (a b) -> p a b", a=4), AF.Copy, [bpt], [bBCs])
            P.dma("sp", s_bc[l], BCs[:].rearrange("p j a b c -> p j (a b c)"), reads=[bBCs], writes=[b_sbc[l]])

        for l in range(1, NL):
            precast(l)
        print("SBUF bytes/partition at end of setup:", P.sb_bytes)
        if self.cfg.get("stop") == "B":
            P.emit()
            return nc
        P.emit(final=False)
        P.release(setup_mark)
        P.barrier()
        N = TN
        Hb = P.sb("Hb", [128, 4, 64], BF16)
        Pb = Rot(P, "Pb", 3, [128, N + 1])
        Rf = P.sb("Rf", [128, 4, N]); bRf = Buf()
        Kf = P.sb("Kf", [128, 4, N]); bKf = Buf()
        Vf = P.sb("Vf", [128, 4, N]); bVf = Buf()
        VF = P.sb("VF", [128, 4, N]); bVF = Buf()
        WA = P.sb("WA", [128, N]); bWA = Buf()
        TWb = P.sb("TWb", [128, N], BF16); bTW = Buf()
        SIG = P.sb("SIG", [128, 4, N]); bSIG = Buf()
        Aa = P.sb("Aa", [128, 4, N], BF16); bAa = Buf()
        WA2 = P.sb("WA2", [128, RW], BF16); bWA2 = Buf()
        V1s = P.sb("V1s", [128, 4, 32], BF16); bV1 = Buf()
        V2s = P.sb("V2s", [32, RW], BF16); bV2 = Buf()
        Vb = P.sb("Vb", [128, 4, N], BF16); bVb = Buf()
        T32 = P.sb("T32", [32, N], BF16); bT32 = Buf()
        G1 = P.sb("G1", [128, 4, N], BF16); bG1 = Buf()
        G2 = P.sb("G2", [128, 4, N], BF16); bG2 = Buf()
        Ub = P.sb("Ub", [128, 4, N], BF16); bUb = Buf()
        tmp = Rot(P, "tmp", 6, [128, N])
        bOPS = [Buf() for _ in range(4)]
        GL = P.sb("GL", [128, 4, N // 64]); bGL = Buf()
        RRV = P.sb("RRV", [128, 4, N]); bRRV = Buf()
        BRK = P.sb("BRK", [128, 4, 128], BF16); bBRK = Buf()
        YW = SIG; bYW = bSIG
        YSf = P.sb("YSf", [128, 4, N]); bYS2 = Buf()
        WGL = P.sb("WGL", [128, 4, RW], BF16); bWGL = Buf()
        BCt = Rot(P, "BCt", 2, [128, 4, 4, 128], BF16)
        YGb = P.sb("YGb", [128, 4, N], BF16); bYGb = Buf()
        xin = Rot(P, "xin", 2, [128, D])
        full = dict(Rf=Rf, Kf=Kf, Vf=Vf, VF=VF, WA=WA, TWb=TWb, SIG=SIG, Aa=Aa, Vb=Vb, T32=T32, G1=G1, G2=G2, Ub=Ub,
                    RRV=RRV, YGb=YGb)
        prompt_mark = P.mark()
        BI = P.sb("BI", [128, 4, N], BF16)
        KI = P.sb("KI", [128, 4, N], BF16)
        KR = P.sb("KR", [128, 4, 2, N], BF16)
        full.update(BI=BI, KI=KI, KR=KR)
        carry = P.sb("carry", [128, DEPTH, 13])
        bcarry = Buf()
        self.memset(carry[:], 0.0, [bcarry])
        Hst = P.sb("Hst", [128, DEPTH, 4, 64])
        bH = Buf()
        self.memset(Hst[:], 0.0, [bH])
        G0 = P.sb("G0", [128, DEPTH, 2, 16])
        bG0 = Buf()
        self.memset(G0[:], 0.0, [bG0])
        NP = N // 128
        NCH = N // 64
        TM = P.sb("TM", [128, NCH, 3, 4, 64], BF16)
        bTM = [Buf() for _ in range(NCH)]
        ABm = P.sb("ABm", [128, NCH, 4, 128], BF16)
        AKm = P.sb("AKm", [128, NCH, 4, 128], BF16)
        Qa = P.sb("Qa", [128, 1, 2, 2, 4, 64], BF16)
        QTa = P.sb("QTa", [128, 1, 2, 2, 4, 64], BF16)
        Rr = P.sb("Rr", [128, 1, 2, 4, 64])
        Rbb = P.sb("Rbb", [128, NCH, 4, 64], BF16)
        bNEs = [Buf(), Buf()]
        bAB = [Buf() for _ in range(NP)]
        bRbb = [Buf() for _ in range(NP)]
        RHSb = P.sb("RHSb", [128, 4, 64], BF16); bRHS = Buf()
        UUb = P.sb("UUb", [128, 4, 64], BF16); bUU = Buf()
        htmp = P.sb("htmp", [128, 4, 64]); bht = Buf()
        CSl = P.sb("CSl", [128, 2, 16, 129]); bCSl = Buf()
        s5a = Rot(P, "s5a", 8, [128, N])
        s5g = Rot(P, "s5g", 2, [128, 2, N])
        s5h = Rot(P, "s5h", 2, [128, 2, N], BF16)
        gtmp = P.sb("gtmp", [128, 8]); bgt = Buf()
        KTl = P.sb("KTl", [128, 8, MEM], BF16); bKTl = Buf()
        VMl = P.sb("VMl", [128, 2, D], BF16); bVMl = Buf()
        ETb = Rot(P, "ETb", 2, [128, 2, N], BF16)
        rden = Rot(P, "rden", 2, [128, N])
        S5O = P.sb("S5O", [128, 2, 16]); bS5O = Buf()
        s5o2 = P.sb("s5o2", [16, 2, 128]); bs5o2 = Buf()
        wko = P.sb("wko", [64, 4, 128]); bwko = Buf()
        shs = P.sb("shs", [16, 128]); bshs = Buf()
        print("SBUF bytes/partition so far:", P.sb_bytes)

        def load_x(t0):
            for s in range(N // 128):
                xi, bxi = xin.next()
                P.dma("sp", xi[:], xp[t0 + s * 128: t0 + (s + 1) * 128, :], writes=[bxi])
                for half in range(2):
                    pt, bpt = PS[2 + half], BPS[2 + half]
                    for q in range(4):
                        kc = half * 4 + q
                        self.tr(pt[:, q * 128:(q + 1) * 128], xi[:, kc * 128:(kc + 1) * 128], identf[:], [bxi, bc], [bpt])
                    self.cp(xT[:, half * 4:(half + 1) * 4, s * 128:(s + 1) * 128], pt[:].rearrange("p (q m) -> p q m", q=4), [bpt], [bxT], eng="act")

        def store_y(t0):
            self.act(SCRB[:, :, 0:N], xT[:, :, 0:N], AF.Square, [bxT], [bSCR])
            pt, bpt = pdense()
            for kc in range(8):
                self.mm(pt[:, 0:N], onesb[:], SCRB[:, kc, 0:N], kc == 0, kc == 7, [bSCR, bc], [bpt])
            self.act(rstd[:, 0:N], pt[:, 0:N], AF.Ln, [bpt], [brstd], bias=NORM_EPS, scale=1.0 / D)
            self.act(rstd[:, 0:N], rstd[:, 0:N], AF.Exp, [brstd], [brstd], scale=-0.5)
            for kc in range(8):
                self.stt(xT[:, kc, 0:N], xT[:, kc, 0:N], pc("norm_f", 0, kc), rstd[:, 0:N], ALU.mult, ALU.mult, [bxT, brstd, bprm], [bxT])
            for s in range(N // 128):
                xi, bxi = xin.next()
                for half in range(2):
                    pt, bpt = PS[2 + half], BPS[2 + half]
                    for q in range(4):
                        kc = half * 4 + q
                        self.tr(pt[:, q * 128:(q + 1) * 128], xT[:, kc, s * 128:(s + 1) * 128], identf[:], [bxT, bc], [bpt])
                    self.cp(xi[:, half * 512:(half + 1) * 512], pt[:], [bpt], [bxi], eng="act")
                P.dma("sp", yp[t0 + s * 128: t0 + (s + 1) * 128, :], xi[:], reads=[bxi], final=True)

        def smp_load_shift(l):
            xa, bxa = xin.next()
            xb_, bxb_ = xin.next()
            P.dma("sp", xa[0:16, 0:1024], st_shift[l, :, 0:1024], writes=[bxa])
            P.dma("sp", xb_[0:16, 0:640], st_shift[l, :, 1024:1664], writes=[bxb_])
            pt, bpt = PS[2], BPS[2]
            for j in range(13):
                src = xa[0:16, j * 128:(j + 1) * 128] if j < 8 else xb_[0:16, (j - 8) * 128:(j - 7) * 128]
                self.tr(pt[:, j * 16:(j + 1) * 16], src, identf[0:16, 0:16], [bxa, bxb_, bc], [bpt])
            self.cp(SSH[:, :, :], pt[:, 0:208].rearrange("p (j b) -> p j b", j=13), [bpt], [bSSH], eng="act")

        def smp_store_shift(l):
            xa, bxa = xin.next()
            xb_, bxb_ = xin.next()
            for g in range(4):
                pt, bpt = PS[2 + g % 2], BPS[2 + g % 2]
                js = list(range(g * 4, min(g * 4 + 4, 13)))
                for qi, j in enumerate(js):
                    self.tr(pt[0:16, qi * 128:(qi + 1) * 128], NSH[:, j, :], identf[:], [bNSH, bc], [bpt])
                w_ = len(js) * 128
                if g < 2:
                    self.cp(xa[0:16, g * 512:g * 512 + w_], pt[0:16, 0:w_], [bpt], [bxa], eng="act")
                else:
                    self.cp(xb_[0:16, (g - 2) * 512:(g - 2) * 512 + w_], pt[0:16, 0:w_], [bpt], [bxb_], eng="act")
            P.dma("sp", o_sshift[l, :, 0:1024], xa[0:16, 0:1024], reads=[bxa], final=True)
            P.dma("sp", o_sshift[l, :, 1024:1664], xb_[0:16, 0:640], reads=[bxb_], final=True)

        def smp_bounce_in(c, srcs):
            tms, btms = TMS.next()
            for g3 in range(2):
                pt, bpt = PS[2 + g3], BPS[2 + g3]
                for qq in range(3):
                    src, bsrc = srcs[g3 * 3 + qq]
                    self.tr(pt[0:64, qq * 128:(qq + 1) * 128], src, identf[:], [bsrc, bc], [bpt])
                self.cp(tms[:, g3 * 3:(g3 + 1) * 3, :], pt[0:64, 0:384].rearrange("p (q m) -> p q m", q=3), [bpt], [btms], eng="act")
            P.dma("sp", s_b1[:, :, c * 128:(c + 1) * 128].rearrange("q n m -> n q m"), tms[:, :, :], reads=[btms], writes=[bsb1])

        def smp_wkv(l, YW):
            for q in range(6):
                P.dma("sp", X6[:, q, :, :], s_b1[q].rearrange("(t b) (h j) -> (b h) t j", t=4, h=8), reads=[bsb1], writes=[bX6s[q % 3]])
            P.dma("sp", Ssm[:].rearrange("p i j -> p (i j)"), st_wkv[l].rearrange("b h i j -> (b h) (i j)"), writes=[bSlo, bShi], sembuf=bSlo)

            def bj(a):
                return a.unsqueeze(1).to_broadcast([128, 64, 64])

            def bi_(a):
                return a.unsqueeze(2).to_broadcast([128, 64, 64])
            SPL = 48
            halves = ((slice(0, SPL), "dve", bSlo, bT1lo), (slice(SPL, 64), "pool", bShi, bT1hi))

            def bj(a, n_):
                return a.unsqueeze(1).to_broadcast([128, n_, 64])

            def bi_(a, n_):
                return a.unsqueeze(2).to_broadcast([128, n_, 64])
            for t in range(4):
                r_, w_, k_, v_, kk_, b_ = [X6[:, q, t, :] for q in range(6)]
                for hs_, eng_, bS_, bT_ in halves:
                    n_ = hs_.stop - hs_.start
                    self.tt(T1s[:, hs_, :], Ssm[:, hs_, :], bj(kk_, n_), ALU.mult, [bS_, *bX6s], [bT_], eng=eng_)
                P.op("dve", lambda e: e.tensor_reduce(out=sks[:], in_=T1s[:], axis=mybir.AxisListType.X, op=ALU.add), reads=[bT1lo, bT1hi], writes=[bsk])
                for hs_, eng_, bS_, bT_ in halves:
                    n_ = hs_.stop - hs_.start
                    self.tt(Ssm[:, hs_, :], Ssm[:, hs_, :], bj(w_, n_), ALU.mult, [bS_, *bX6s], [bS_], eng=eng_)
                for hs_, eng_, bS_, bT_ in halves:
                    n_ = hs_.stop - hs_.start
                    self.tt(T1s[:, hs_, :], bi_(sks[:, hs_], n_), bj(b_, n_), ALU.mult, [bsk, *bX6s], [bT_], eng=eng_)
                    self.tt(Ssm[:, hs_, :], Ssm[:, hs_, :], T1s[:, hs_, :], ALU.subtract, [bS_, bT_], [bS_], eng=eng_)
                for hs_, eng_, bS_, bT_ in halves:
                    n_ = hs_.stop - hs_.start
                    self.tt(T1s[:, hs_, :], bi_(v_[:, hs_], n_), bj(k_, n_), ALU.mult, [*bX6s], [bT_], eng=eng_)
                    self.tt(Ssm[:, hs_, :], Ssm[:, hs_, :], T1s[:, hs_, :], ALU.add, [bS_, bT_], [bS_], eng=eng_)
                for hs_, eng_, bS_, bT_ in halves:
                    n_ = hs_.stop - hs_.start
                    self.tt(T1s[:, hs_, :], Ssm[:, hs_, :], bj(r_, n_), ALU.mult, [bS_, *bX6s], [bT_], eng=eng_)
                P.op("dve", lambda e, t=t: e.tensor_reduce(out=Ysm[:, t, :], in_=T1s[:], axis=mybir.AxisListType.X, op=ALU.add), reads=[bT1lo, bT1hi], writes=[bYsm])
            P.dma("sp", o_swkv[l].rearrange("b h i j -> (b h) (i j)"), Ssm[:].rearrange("p i j -> p (i j)"), reads=[bSlo, bShi], sembuf=bSlo, final=True)
            P.dma("sp", s_b2.rearrange("(t b) (h i) -> (b h) t i", t=4, h=8), Ysm[:], reads=[bYsm], writes=[bsb2])
            P.dma("sp", ytm[:], s_b2[:, :], reads=[bsb2], writes=[bytm])
            pt, bpt = PS[2], BPS[2]
            for c in range(4):
                self.tr(pt[:, c * 64:(c + 1) * 64], ytm[:, c * 128:(c + 1) * 128], identf[0:64, 0:64], [bytm, bc], [bpt])
            self.cp(YW[:, :, :], pt[:, 0:256].rearrange("p (c m) -> p c m", c=4), [bpt], [bSIG], eng="act")

        def smp_s5_recur(l, Ub):
            for ri, src in enumerate((st_s5re, st_s5im)):
                for hf in range(2):
                    xa, bxa = xin.next()
                    P.dma("sp", xa[0:16, :], src[l, :, hf * 1024:(hf + 1) * 1024], writes=[bxa])
                    pt, bpt = PS[2 + hf], BPS[2 + hf]
                    for k8 in range(8):
                        self.tr(pt[:, k8 * 16:(k8 + 1) * 16], xa[0:16, k8 * 128:(k8 + 1) * 128], identf[0:16, 0:16], [bxa, bc], [bpt])
                    self.cp(Hs5[:, 0, ri, hf * 8:(hf + 1) * 8, :], pt[:, 0:128].rearrange("p (k b) -> p k b", k=8), [bpt], [bHs5], eng="act")
            for jc in range(4):
                bct, bbct = BCt.next()
                P.dma("sp", bct[:].rearrange("p a b c -> p (a b c)"), s_bc[l, :, jc, :], reads=[b_sbc[l]], writes=[bbct])
                for kk in range(4):
                    k = jc * 4 + kk
                    for ri in range(2):
                        bk = 2 + ri * 2 + k // 8
                        self.mm(PS[bk][:, (k % 8) * 64:(k % 8 + 1) * 64], bct[:, kk, ri, :], Ub[:, jc, :], True, True, [bbct, bUb], [BPS[bk]])
            lbr = LBR[:, l * 16:(l + 1) * 16].unsqueeze(2).to_broadcast([128, 16, 16])
            lbi = LBI[:, l * 16:(l + 1) * 16].unsqueeze(2).to_broadcast([128, 16, 16])
            rr_ = [bHs5, bS5P, bs5t]
            for t in range(4):
                cur, nxt = t % 2, (t + 1) % 2
                hre, him = Hs5[:, cur, 0], Hs5[:, cur, 1]
                nre, nim = Hs5[:, nxt, 0], Hs5[:, nxt, 1]
                self.tt(s5t[:, 0], him, lbi, ALU.mult, rr_, [bs5t])
                self.tt(s5t[:, 1], hre, lbi, ALU.mult, rr_, [bs5t])
                self.tt(nre, hre, lbr, ALU.mult, rr_, [bHs5])
                self.tt(nim, him, lbr, ALU.mult, rr_, [bHs5])
                self.tt(nre, nre, s5t[:, 0], ALU.subtract, rr_, [bHs5])
                self.tt(nim, nim, s5t[:, 1], ALU.add, rr_, [bHs5])
                for ri, dst in ((0, nre), (1, nim)):
                    for hf in range(2):
                        bk = 2 + ri * 2 + hf
                        bu = PS[bk][:, :].rearrange("p (k n) -> p k n", k=8)[:, :, t * 16:(t + 1) * 16]
                        self.tt(dst[:, hf * 8:(hf + 1) * 8, :], dst[:, hf * 8:(hf + 1) * 8, :], bu, ALU.add, [bHs5, BPS[bk]], [bHs5])
                self.cp(HsT[:, 0, :, t * 16:(t + 1) * 16], nre, [bHs5], [bHsT], eng="pool")
                self.cp(HsT[:, 1, :, t * 16:(t + 1) * 16], nim, [bHs5], [bHsT], eng="pool")
            for ri, dstd in ((0, o_ss5re), (1, o_ss5im)):
                for g in range(4):
                    pt, bpt = PS[6], BPS[6]
                    for kk in range(4):
                        self.tr(pt[0:16, kk * 128:(kk + 1) * 128], Hs5[:, 0, ri, g * 4 + kk, :], identf[:], [bHs5, bc], [bpt])
                    xa, bxa = xin.next()
                    self.cp(xa[0:16, 0:512], pt[0:16, :], [bpt], [bxa], eng="act")
                    P.dma("sp", dstd[l, :, g * 512:(g + 1) * 512], xa[0:16, 0:512], reads=[bxa], final=True)

        def smp_attn(l):
            pdn, bpdn = PS[5], BPS[5]
            po, bpo = PS[6], BPS[6]
            t1flat = T1s[:].rearrange("p i j -> p (i j)")
            Kalt = t1flat[:, 0:2048].rearrange("p (a d) -> p a d", a=2)
            Valt = t1flat[:, 3072:4096].bitcast(BF16).rearrange("p (a d) -> p a d", a=2)
            for b in range(NB_S):
                if b % 2 == 0:
                    Kb_, bKb_, Vb_, bVb_ = Kf32, [bKf32], Vs, [bVs]
                else:
                    Kb_, bKb_, Vb_, bVb_ = Kalt, [bT1lo], Valt, [bT1hi]
                P.dma("sp", Kb_[:, :, :], ck[l, b].rearrange("(mc p) d -> p mc d", p=128), writes=bKb_)
                P.dma("pool", Vb_[:, :, :], cv[l, b].rearrange("(mc p) d -> p mc d", p=128), writes=bVb_)
                for mc in range(2):
                    for g4 in range(2):
                        pt, bpt = PS[2 + g4], BPS[2 + g4]
                        for q in range(4):
                            dc = g4 * 4 + q
                            self.tr(pt[:, q * 128:(q + 1) * 128], Kb_[:, mc, dc * 128:(dc + 1) * 128], identf[:], bKb_ + [bc], [bpt])
                        self.cp(KTs[:, g4 * 4:(g4 + 1) * 4, mc * 128:(mc + 1) * 128], pt[:].rearrange("p (q m) -> p q m", q=4), [bpt], [bKTs], eng="act")
                psc, bpsc = (PS[4], BPS[4]) if b % 2 == 0 else (PS[7], BPS[7])
                for mc in range(2):
                    for h in range(4):
                        c0_ = (mc * 4 + h) * 4
                        for dcl in range(2):
                            dc = 2 * h + dcl
                            self.mm(psc[:, c0_:c0_ + 4], KTs[:, dc, mc * 128:(mc + 1) * 128], SCRB[:, dc, b:64:16], dcl == 0, dcl == 1, [bKTs, bSCR], [bpsc])
                self.act(ETs[:, b, :], psc[:, 0:32], AF.Exp, [bpsc], [bETs], scale=1.0 / 16.0)
                for mc in range(2):
                    self.mm(pdn[:, b * 16:(b + 1) * 16], onesb[:], ETs[:, b, mc * 16:(mc + 1) * 16], mc == 0, mc == 1, [bETs, bc], [bpdn])
                for dc in range(8):
                    h = dc // 2
                    for mc in range(2):
                        c0_ = (mc * 4 + h) * 4
                        self.mm(po[:, dc * 64 + b:dc * 64 + 64:16], Vb_[:, mc, dc * 128:(dc + 1) * 128], ETs[:, b, c0_:c0_ + 4], mc == 0, mc == 1, bVb_ + [bETs], [bpo])
            self.act(rds[:, :, :], pdn[:, 0:256].rearrange("p (b x) -> p b x", b=16), AF.Ln, [bpdn], [brds])
            self.act(rds[:, :, :], rds[:, :, :], AF.Exp, [brds], [brds], scale=-1.0)
            for dc in range(8):
                h = dc // 2
                self.tt(ACTB[:, dc, 0:64].rearrange("p (t b) -> p t b", t=4), po[:, dc * 64:(dc + 1) * 64].rearrange("p (t b) -> p t b", t=4),
                        rds[:, :, h * 4:(h + 1) * 4].rearrange("p b t -> p t b"), ALU.mult, [bpo, brds], [bACT])

        def layer(l, last_tile, N, smp):
            Rf, Kf, Vf, VF, SIG, Aa, Vb, G1, G2, Ub, RRV, YGb = [full[n_][:, :, 0:N] for n_ in ("Rf", "Kf", "Vf", "VF", "SIG", "Aa", "Vb", "G1", "G2", "Ub", "RRV", "YGb")]
            WA, TWb, T32 = full["WA"][:, 0:N], full["TWb"][:, 0:N], full["T32"][:, 0:N]
            YW = SIG
            YS = YSf[:, :, 0:N]
            if not smp:
                BI, KI = full["BI"][:, :, 0:N], full["KI"][:, :, 0:N]
                KR = full["KR"][:, :, :, 0:N]

            def tnext():
                t_, b_ = tmp.next()
                return t_[:, 0:N], b_
            rmsnorm("norm_mix", l, N)
            if smp:
                smp_load_shift(l)
            P.dma("pool", WA2[0:64, :], w2[l], writes=[bWA2])
            P.dma("pool", WA2[64:128, :], a2[l], writes=[bWA2])
            if l > 0:
                P.dma("pool", V1s[:], v1[l - 1].rearrange("(c p) r -> p c r", p=128), writes=[bV1])
                P.dma("pool", V2s[:], v2[l - 1], writes=[bV2])
            P.dma("pool", WGL[:], wglu[l].rearrange("(c p) r -> p c r", p=128), writes=[bWGL])
            if not smp:
                P.dma("sp", CSl[:].rearrange("p a k t -> p (a k t)"), s_cs[l], reads=[b_scs[l]], writes=[bCSl])
            vdst, bvdst = (VF, bVF) if l == 0 else (Vf, bVf)
            def g_proj(order):
                for grp in order:
                    wt, bw = wload_t(s_win[l, grp], 640, b_swin[l])
                    for jj in range(5):
                        j = grp * 5 + jj
                        pt, bpt = pdense()
                        for kc in range(8):
                            self.mm(pt[:, 0:N], wt[:, kc, jj * 128:(jj + 1) * 128], ACTB[:, kc, 0:N], kc == 0, kc == 7, [bw, bACT], [bpt])
                        if j < 13:
                            pb, bpb = Pb.next()
                            if not smp:
                                self.cp(pb[:, 0:1], carry[:, l, j:j + 1], [bcarry], [bpb], eng="pool")
                                self.cp(pb[:, 1:N + 1], pt[:, 0:N], [bpt], [bpb], eng="act")
                                self.cp(carry[:, l, j:j + 1], pb[:, N:N + 1], [bpb], [bcarry], eng="pool")
                                prev_, cur_ = pb[:, 0:N], pb[:, 1:N + 1]
                            else:
                                self.cp(pb[:, 0:16], SSH[:, j, :], [bSSH], [bpb], eng="pool")
                                self.cp(pb[:, 16:80], pt[:, 0:64], [bpt], [bpb], eng="act")
                                self.cp(NSH[:, j, :], pb[:, 64:80], [bpb], [bNSH], eng="pool")
                                prev_, cur_ = pb[:, 0:64], pb[:, 16:80]
                            tm_, btm_ = tnext()
                            self.act(tm_[:, 0:N], prev_, AF.Identity, [bpb, bprm], [btm_], scale=pc("mu_shift", l, j))
                            if j < 4:
                                dst, bd = Rf[:, j, :], bRf
                            elif j < 8:
                                dst, bd = Kf[:, j - 4, :], bKf
                            elif j < 12:
                                dst, bd = vdst[:, j - 8, :], bvdst
                            else:
                                dst, bd = WA[:, :], bWA
                            self.stt(dst, cur_, OMM[:, l * 13 + j:l * 13 + j + 1], tm_[:, 0:N], ALU.mult, ALU.add, [bpb, btm_, bprm], [bd])
                        elif j < 17:
                            self.act(G1[:, j - 13, :], pt[:, 0:N], AF.Silu, [bpt], [bG1])
                        elif j < 21:
                            self.act(Ub[:, j - 17, :], pt[:, 0:N], AF.Copy, [bpt], [bUb])
                        else:
                            self.act(G2[:, j - 21, :], pt[:, 0:N], AF.Silu, [bpt], [bG2])
                        yield
            def g_rwkv():
                self.act(TWb[0:64, :], WA[0:64, :], AF.Tanh, [bWA], [bTW])
                self.cp(TWb[64:128, :], WA[64:128, :], [bWA], [bTW], eng="pool")
                for c in range(4):
                    pt, bpt = pdense()
                    self.mm(pt[:, 0:N], WA2[0:64, c * 128:(c + 1) * 128], TWb[0:64, :], True, True, [bWA2, bTW], [bpt])
                    self.act(SIG[:, c, :], pt[:, 0:N], AF.Sigmoid, [bpt, bprm], [bSIG], bias=pc("w0", l, c))
                    pt, bpt = pdense()
                    self.mm(pt[:, 0:N], WA2[64:128, c * 128:(c + 1) * 128], TWb[64:128, :], True, True, [bWA2, bTW], [bpt])
                    self.act(Aa[:, c, :], pt[:, 0:N], AF.Sigmoid, [bpt, bprm], [bAa], bias=pc("a0", l, c))
                yield
                if l > 0:
                    self.cp(Vb[:, :, :], Vf[:, :, :], [bVf], [bVb], eng="pool")
                    pt, bpt = pdense()
                    for c in range(4):
                        self.mm(pt[0:32, 0:N], V1s[:, c, :], Vb[:, c, :], c == 0, c == 3, [bV1, bVb], [bpt])
                    self.cp(T32[:, :], pt[0:32, 0:N], [bpt], [bT32], eng="act")
                    for c in range(4):
                        pt, bpt = pdense()
                        self.mm(pt[:, 0:N], V2s[0:32, c * 128:(c + 1) * 128], T32[0:32, :], True, True, [bV2, bT32], [bpt])
                        g_, bg_ = tnext()
                        self.act(g_[:, :], pt[:, 0:N], AF.Sigmoid, [bpt, bprm], [bg_], bias=pc("v0", l - 1, c))
                        d_, bd_ = tnext()
                        self.tt(d_[:, :], VF[:, c, :], Vf[:, c, :], ALU.subtract, [bVF, bVf], [bd_])
                        self.tt(d_[:, :], d_[:, :], g_[:, :], ALU.mult, [bd_, bg_], [bd_])
                        self.tt(Vf[:, c, :], Vf[:, c, :], d_[:, :], ALU.add, [bVf, bd_], [bVf])
                self.cp(Vb[:, :, :], vdst[:, :, :], [bvdst], [bVb], eng="pool")
                yield
                for c in range(4):
                    self.ts(BRK[:, c, :], bonesb[:], pc("r_k", l, c), ALU.mult, [bc, bprm], [bBRK], eng="pool")
                for c in range(4):
                    bo = bOPS[c]
                    kkr, bkkr = tnext()
                    self.act(kkr[:, :], Kf[:, c, :], AF.Identity, [bKf, bprm], [bkkr], scale=pc("k_k", l, c))
                    sq, bsq = tnext()
                    self.act(sq[:, :], kkr[:, :], AF.Square, [bkkr], [bsq])
                    pt, bpt = pdense()
                    self.mm(pt[:, 0:N], bonesf[:], sq[:, :], True, True, [bsq, bc], [bpt])
                    self.act(sq[:, :], pt[:, 0:N], AF.Ln, [bpt], [bsq], scale=64.0, bias=1e-12)
                    self.act(sq[:, :], sq[:, :], AF.Exp, [bsq], [bsq], scale=-0.5)
                    self.tt(kkr[:, :], kkr[:, :], sq[:, :], ALU.mult, [bkkr, bsq], [bkkr])
                    bq, bbq = tnext()
                    self.tt(bq[:, :], kkr[:, :], Aa[:, c, :], ALU.mult, [bkkr, bAa], [bbq])
                    kp, bkp = tnext()
                    self.act(kp[:, :], Aa[:, c, :], AF.Identity, [bAa, bprm], [bkp], scale=pc("k_a", l, c), bias=OMK[:, l * 4 + c:l * 4 + c + 1])
                    self.tt(kp[:, :], kp[:, :], Kf[:, c, :], ALU.mult, [bkp, bKf], [bkp])
                    rk, brk = tnext()
                    self.tt(sq[:, :], Rf[:, c, :], kp[:, :], ALU.mult, [bRf, bkp], [bsq])
                    self.cp(TWb[:, :], sq[:, :], [bsq], [bTW], eng="pool")
                    pt, bpt = pdense()
                    self.mm(pt[:, 0:N], BRK[:, c, :], TWb[:, :], True, True, [bBRK, bTW], [bpt])
                    self.tt(RRV[:, c, :], pt[:, 0:N], vdst[:, c, :], ALU.mult, [bpt, bvdst], [bRRV])
                    if smp:
                        gam, bgam = rk, brk
                        self.act(gam[:, :], SIG[:, c, :], AF.Exp, [bSIG], [bgam], scale=-C0)
                        smp_bounce_in(c, [(Rf[:, c, :], bRf), (gam[:, :], bgam), (kp[:, :], bkp), (vdst[:, c, :], bvdst), (kkr[:, :], bkkr), (bq[:, :], bbq)])
                        yield
                        continue
                    cum, bcum = rk, brk
                    self.scan(cum[:, :], cmask[:, 0:N], SIG[:, c, :], 0.0, [bc, bSIG], [bcum])
                    gam, bgam = sq, bsq
                    self.act(gam[:, :], cum[:, :], AF.Exp, [bcum], [bgam], scale=-C0)
                    self.cp(GL[:, c, :], gam[:, :].rearrange("p (a b) -> p a b", b=64)[:, :, 63], [bgam], [bGL], eng="pool")
                    self.tt(KR[:, c, 1, :], Rf[:, c, :], gam[:, :], ALU.mult, [bRf, bgam], [bo])
                    self.tt(gam[:, :], cum[:, :], SIG[:, c, :], ALU.subtract, [bcum, bSIG], [bgam])
                    self.act(gam[:, :], gam[:, :], AF.Exp, [bgam], [bgam], scale=-C0)
                    self.tt(KR[:, c, 0, :], kkr[:, :], gam[:, :], ALU.mult, [bkkr, bgam], [bo])
                    self.act(gam[:, :], cum[:, :], AF.Exp, [bcum], [bgam], scale=C0)
                    self.tt(BI[:, c, :], bq[:, :], gam[:, :], ALU.mult, [bbq, bgam], [bo])
                    self.tt(KI[:, c, :], kp[:, :], gam[:, :], ALU.mult, [bkp, bgam], [bo])
                    yield
                if smp:
                    smp_wkv(l, YW)
                for ch in range(0 if smp else NCH):
                    cs = slice(ch * 64, (ch + 1) * 64)
                    pt, bpt = pdense()
                    ptb = pt[:].bitcast(BF16)
                    for h2 in (0, 1):
                        hb = h2 * 64
                        P.pe_fence()
                        for xi, (src, bsrc) in enumerate(((Vb, [bVb]), (BI, bOPS), (KI, bOPS))):
                            for c in range(4):
                                cl = (xi * 4 + c) * 64
                                self.tr(ptb[hb:hb + 64, cl:cl + 64], src[hb:hb + 64, c, cs], identb[hb:hb + 64, hb:hb + 64], list(bsrc) + [bc], [bpt])
                    self.cp(TM[:, ch, :, :, :], ptb[:, 0:768].rearrange("p (x c m) -> p x c m", x=3, c=4), [bpt], [bTM[ch]], eng="act")
                    yield
                for pp in range(0 if smp else NP):
                    st = 0
                    bne = bNEs[st]
                    for par in range(2):
                        ch = pp * 2 + par
                        cs = slice(ch * 64, (ch + 1) * 64)
                        pab, bpab = PS[2], BPS[2]
                        pak, bpak = PS[3], BPS[3]
                        pq, bpq = PS[4], BPS[4]
                        for h2 in (0, 1):
                            hb = h2 * 64
                            P.pe_fence()
                            for c in range(4):
                                self.mm(pab[hb:hb + 64, c * 128:(c + 1) * 128], BI[hb:hb + 64, c, cs], KR[hb:hb + 64, c, :, cs], True, True, [bOPS[c]], [bpab])
                                self.mm(pak[hb:hb + 64, c * 128:(c + 1) * 128], KI[hb:hb + 64, c, cs], KR[hb:hb + 64, c, :, cs], True, True, [bOPS[c]], [bpak])
                                self.mm(pq[hb:hb + 64, c * 64:(c + 1) * 64], KR[hb:hb + 64, c, 0, cs], BI[hb:hb + 64, c, cs], True, True, [bOPS[c]], [bpq])
                        mb = mska[:, :].unsqueeze(1).to_broadcast([128, 4, 128])
                        self.tt(ABm[:, ch, :, :], pab[:, :].rearrange("p (c m) -> p c m", c=4), mb, ALU.mult, [bpab, bc], [bAB[pp]])
                        self.tt(AKm[:, ch, :, :], pak[:, :].rearrange("p (c m) -> p c m", c=4), mb, ALU.mult, [bpak, bc], [bAB[pp]])
                        ml = mskl[:, :].unsqueeze(1).to_broadcast([128, 4, 64])
                        self.stt(QTa[:, st, 0, par, :, :], pq[:, 0:256].rearrange("p (c m) -> p c m", c=4), -1.0, ml, ALU.mult, ALU.mult, [bpq, bc], [bne])
                        self.ts(Qa[:, st, 0, par, :, :], ABm[:, ch, :, 0:64], -1.0, ALU.mult, [bAB[pp]], [bne])
                        for tb in (0, 64):
                            idb = identf[tb:tb + 64, tb:tb + 64].unsqueeze(1).to_broadcast([64, 4, 64])
                            self.tt(Rr[tb:tb + 64, st, par, :, :], Qa[tb:tb + 64, st, 0, par, :, :], idb, ALU.add, [bne, bc], [bne])
                        yield
                    chs = slice(pp * 2, pp * 2 + 2)
                    self.cp(Rbb[:, chs, :, :], Rr[:, st, :, :, :], [bne], [bRbb[pp]], eng="pool")

                    def v4(ap):
                        return ap.rearrange("p (a c m) -> p a c m", a=2, c=4)
                    for lev in range(1, 6):
                        a_, b_ = (lev - 1) % 2, lev % 2
                        pq1, bpq1 = PS[2], BPS[2]
                        pq2, bpq2 = PS[3], BPS[3]
                        pq3, bpq3 = PS[4], BPS[4]
                        for h2 in (0, 1):
                            hb = h2 * 64
                            P.pe_fence()
                            for par in range(2):
                                for c in range(4):
                                    hs = slice((par * 4 + c) * 64, (par * 4 + c + 1) * 64)
                                    if lev < 5:
                                        self.mm(pq1[hb:hb + 64, hs], QTa[hb:hb + 64, st, a_, par, c, :], Qa[hb:hb + 64, st, a_, par, c, :], True, True, [bne], [bpq1])
                                    self.mm(pq2[hb:hb + 64, hs], Qa[hb:hb + 64, st, a_, par, c, :], QTa[hb:hb + 64, st, a_, par, c, :], True, True, [bne], [bpq2])
                        if lev < 5:
                            self.cp(Qa[:, st, b_, :, :, :], v4(pq1[:]), [bpq1], [bne], eng="act")
                        self.cp(QTa[:, st, b_, :, :, :], v4(pq2[:]), [bpq2], [bne], eng="act")
                        yield
                        for h2 in (0, 1):
                            hb = h2 * 64
                            P.pe_fence()
                            for par in range(2):
                                for c in range(4):
                                    hs = slice((par * 4 + c) * 64, (par * 4 + c + 1) * 64)
                                    self.mm(pq3[hb:hb + 64, hs], QTa[hb:hb + 64, st, b_, par, c, :], Rbb[hb:hb + 64, pp * 2 + par, c, :], True, True, [bne, bRbb[pp]], [bpq3])
                        self.tt(Rr[:, st, :, :, :], Rr[:, st, :, :, :], v4(pq3[:]), ALU.add, [bne, bpq3], [bne])
                        self.cp(Rbb[:, chs, :, :], Rr[:, st, :, :, :], [bne], [bRbb[pp]], eng="pool")
                        yield
                if not smp:
                    self.cp(Hb[:], Hst[:, l, :, :], [bH], [bH], eng="pool")
                for ch in range(0 if smp else NCH):
                    pp = ch // 2
                    cs = slice(ch * 64, (ch + 1) * 64)
                    pr, bpr = PS[2], BPS[2]
                    for h2 in (0, 1):
                        hb = h2 * 64
                        P.pe_fence()
                        for c in range(4):
                            hs = slice(c * 64, (c + 1) * 64)
                            self.mm(pr[hb:hb + 64, hs], KR[hb:hb + 64, c, 0, cs], Hb[hb:hb + 64, c, :], True, False, [bOPS[c], bH], [bpr])
                            self.mm(pr[hb:hb + 64, hs], AKm[hb:hb + 64, ch, c, 0:64], TM[hb:hb + 64, ch, 0, c, :], False, True, [bAB[pp], bTM[ch]], [bpr])
                    self.act(RHSb[:, :, :], pr[:, 0:256].rearrange("p (c m) -> p c m", c=4), AF.Copy, [bpr], [bRHS], scale=-1.0)
                    yield
                    pu, bpu = PS[3], BPS[3]
                    for h2 in (0, 1):
                        hb = h2 * 64
                        P.pe_fence()
                        for c in range(4):
                            hs = slice(c * 64, (c + 1) * 64)
                            self.mm(pu[hb:hb + 64, hs], Rbb[hb:hb + 64, ch, c, :], RHSb[hb:hb + 64, c, :], True, True, [bRbb[pp], bRHS], [bpu])
                    self.cp(UUb[:, :, :], pu[:, 0:256].rearrange("p (c m) -> p c m", c=4), [bpu], [bUU], eng="act")
                    yield
                    py, bpy = PS[4], BPS[4]
                    ph, bph = PS[4][:, 256:512], BPS[4]
                    for h2 in (0, 1):
                        hb = h2 * 64
                        P.pe_fence()
                        for c in range(4):
                            hs = slice(c * 64, (c + 1) * 64)
                            self.mm(py[hb:hb + 64, hs], Hb[hb:hb + 64, c, :], KR[hb:hb + 64, c, 1, cs], True, False, [bH, bOPS[c]], [bpy])
                            self.mm(py[hb:hb + 64, hs], UUb[hb:hb + 64, c, :], ABm[hb:hb + 64, ch, c, 64:128], False, False, [bUU, bAB[pp]], [bpy])
                            self.mm(py[hb:hb + 64, hs], TM[hb:hb + 64, ch, 0, c, :], AKm[hb:hb + 64, ch, c, 64:128], False, True, [bTM[ch], bAB[pp]], [bpy])
                            self.mm(ph[hb:hb + 64, hs], TM[hb:hb + 64, ch, 1, c, :], UUb[hb:hb + 64, c, :], True, False, [bTM[ch], bUU], [bph])
                            self.mm(ph[hb:hb + 64, hs], TM[hb:hb + 64, ch, 2, c, :], TM[hb:hb + 64, ch, 0, c, :], False, True, [bTM[ch]], [bph])
                    self.cp(YW[:, :, cs], py[:, 0:256].rearrange("p (c m) -> p c m", c=4), [bpy], [bYW], eng="act")
                    self.tt(htmp[:], ph[:, 0:256].rearrange("p (c m) -> p c m", c=4), Hst[:, l, :, :], ALU.add, [bph, bH], [bht])
                    self.tt(Hst[:, l, :, :], htmp[:], GL[:, :, ch:ch + 1].to_broadcast([128, 4, 64]), ALU.mult, [bht, bGL], [bH])
                    self.cp(Hb[:], Hst[:, l, :, :], [bH], [bH], eng="act")
                    yield
                for c in range(4):
                    pt, bpt = pdense()
                    self.mm(pt[:, 0:N], bonesf[:], YW[:, c, :], True, True, [bYW, bc], [bpt])
                    yc, byc = tnext()
                    self.tt(yc[:, :], YW[:, c, :], pt[:, 0:N], ALU.subtract, [bYW, bpt], [byc])
                    sq, bsq = tnext()
                    self.act(sq[:, :], yc[:, :], AF.Square, [byc], [bsq])
                    pt, bpt = pdense()
                    self.mm(pt[:, 0:N], bonesf[:], sq[:, :], True, True, [bsq, bc], [bpt])
                    self.act(sq[:, :], pt[:, 0:N], AF.Ln, [bpt], [bsq], bias=GN_EPS)
                    self.act(sq[:, :], sq[:, :], AF.Exp, [bsq], [bsq], scale=-0.5)
                    self.tt(yc[:, :], yc[:, :], sq[:, :], ALU.mult, [byc, bsq], [byc])
                    self.act(yc[:, :], yc[:, :], AF.Identity, [byc, bprm], [byc], scale=pc("gn_w", l, c), bias=pc("gn_b", l, c))
                    self.tt(yc[:, :], yc[:, :], RRV[:, c, :], ALU.add, [byc, bRRV], [byc])
                    self.tt(ACTB[:, c, 0:N], yc[:, :], G1[:, c, :], ALU.mult, [byc, bG1], [bACT])
                    yield
            def g_s5():
                if smp:
                    smp_s5_recur(l, Ub)
                pyc, bpyc = PS[7], BPS[7]

                def post(jc):
                    self.stt(YS[:, jc, :], Ub[:, jc, :], pc("d_skip", l, jc), pyc[:, 0:N], ALU.mult, ALU.add, [bUb, bpyc, bprm], [bYS2])
                    self.act(YS[:, jc, :], YS[:, jc, :], AF.Gelu, [bYS2], [bYS2])
                    self.cp(YGb[:, jc, :], YS[:, jc, :], [bYS2], [bYGb], eng="pool")
                if smp:
                    for jc in range(4):
                        bct, bbct = BCt.next()
                        P.dma("sp", bct[:].rearrange("p a b c -> p (a b c)"), s_bc[l, :, jc, :], reads=[b_sbc[l]], writes=[bbct])
                        for kk in range(4):
                            k = jc * 4 + kk
                            self.mm(pyc[:, 0:N], bct[:, kk, 2, :], HsT[:, 0, k, :], kk == 0, False, [bbct, bHsT], [bpyc])
                            self.mm(pyc[:, 0:N], bct[:, kk, 3, :], HsT[:, 1, k, :], False, kk == 3, [bbct, bHsT], [bpyc])
                        post(jc)
                        yield
                else:
                    NQ = N // 128

                    def v3(ap):
                        return ap.rearrange("p (q t) -> p q t", t=128)
                    S_ = {}
                    bcts = {}

                    def tabs(k):
                        return (CSl[:, 0, k, 0:128].unsqueeze(1).to_broadcast([128, NQ, 128]),
                                CSl[:, 1, k, 0:128].unsqueeze(1).to_broadcast([128, NQ, 128]))

                    def a_mm(k):
                        jc, kk = divmod(k, 4)
                        if kk == 0:
                            bct, bbct = BCt.next()
                            P.dma("sp", bct[:].rearrange("p a b c -> p (a b c)"), s_bc[l, :, jc, :], reads=[b_sbc[l]], writes=[bbct])
                            bcts[jc] = (bct, bbct)
                        bct, bbct = bcts[jc]
                        bank = 5 + k % 2
                        pbr, pbi, bpb = PS[bank][:, 0:256], PS[bank][:, 256:512], BPS[bank]
                        self.mm(pbr[:, 0:N], bct[:, kk, 0, :], Ub[:, jc, :], True, True, [bbct, bUb], [bpb])
                        self.mm(pbi[:, 0:N], bct[:, kk, 1, :], Ub[:, jc, :], True, True, [bbct, bUb], [bpb])
                        S_[k] = dict(pbr=pbr, pbi=pbi, bpb=bpb, t=[s5a.next() for _ in range(4)], g=s5g.next(), h=s5h.next())

                    def a_dve(k):
                        d = S_[k]
                        cosb, sinb = tabs(k)
                        (t1, bt1), (t2, bt2), (t3, bt3), (t4, bt4) = d["t"]
                        self.tt(v3(t1[:, :]), v3(d["pbr"][:, 0:N]), cosb, ALU.mult, [d["bpb"], bCSl], [bt1])
                        self.tt(v3(t2[:, :]), v3(d["pbi"][:, 0:N]), sinb, ALU.mult, [d["bpb"], bCSl], [bt2])
                        self.tt(v3(t3[:, :]), v3(d["pbi"][:, 0:N]), cosb, ALU.mult, [d["bpb"], bCSl], [bt3])
                        self.tt(v3(t4[:, :]), v3(d["pbr"][:, 0:N]), sinb, ALU.mult, [d["bpb"], bCSl], [bt4])

                    def a_pool(k):
                        (t1, bt1), (t2, bt2), (t3, bt3), (t4, bt4) = S_[k]["t"]
                        self.tt(t1[:, :], t1[:, :], t2[:, :], ALU.add, [bt1, bt2], [bt1], eng="pool")
                        self.tt(t3[:, :], t3[:, :], t4[:, :], ALU.subtract, [bt3, bt4], [bt3], eng="pool")

                    def a_scan(k):
                        d = S_[k]
                        (t1, bt1), (t2, bt2), (t3, bt3), (t4, bt4) = d["t"]
                        g, bg = d["g"]
                        rho = RHO[:, l * 16 + k:l * 16 + k + 1]
                        cr = CSl[:, 0, k, 128:129]
                        ci = CSl[:, 1, k, 128:129]
                        for q in range(NQ):
                            qs = slice(q * 128, (q + 1) * 128)
                            self.scan(g[:, 0, qs], rho.to_broadcast([128, 128]), t1[:, qs], G0[:, l, 0, k:k + 1], [bt1, bS5P, bG0], [bg])
                            self.scan(g[:, 1, qs], rho.to_broadcast([128, 128]), t3[:, qs], G0[:, l, 1, k:k + 1], [bt3, bS5P, bG0], [bg])
                            e1 = q * 128 + 127
                            self.ts(gtmp[:, 0:1], g[:, 1, e1:e1 + 1], ci, ALU.mult, [bg, bCSl], [bgt])
                            self.ts(gtmp[:, 1:2], g[:, 1, e1:e1 + 1], cr, ALU.mult, [bg, bCSl], [bgt])
                            self.stt(G0[:, l, 0, k:k + 1], g[:, 0, e1:e1 + 1], cr, gtmp[:, 0:1], ALU.mult, ALU.subtract, [bg, bgt, bCSl], [bG0])
                            self.stt(G0[:, l, 1, k:k + 1], g[:, 0, e1:e1 + 1], ci, gtmp[:, 1:2], ALU.mult, ALU.add, [bg, bgt, bCSl], [bG0])

                    def b_pool(k):
                        d = S_[k]
                        cosb, sinb = tabs(k)
                        (t1, bt1), (t2, bt2), (t3, bt3), (t4, bt4) = d["t"]
                        g, bg = d["g"]
                        self.tt(v3(t1[:, :]), v3(g[:, 0, :]), cosb, ALU.mult, [bg, bCSl], [bt1], eng="pool")
                        self.tt(v3(t2[:, :]), v3(g[:, 1, :]), sinb, ALU.mult, [bg, bCSl], [bt2], eng="pool")
                        self.tt(v3(t3[:, :]), v3(g[:, 0, :]), sinb, ALU.mult, [bg, bCSl], [bt3], eng="pool")
                        self.tt(v3(t4[:, :]), v3(g[:, 1, :]), cosb, ALU.mult, [bg, bCSl], [bt4], eng="pool")

                    def b_dve(k):
                        d = S_[k]
                        jc, kk = divmod(k, 4)
                        bct, bbct = bcts[jc]
                        (t1, bt1), (t2, bt2), (t3, bt3), (t4, bt4) = d["t"]
                        hh_, bhh = d["h"]
                        self.tt(hh_[:, 0, :], t1[:, :], t2[:, :], ALU.subtract, [bt1, bt2], [bhh])
                        self.tt(hh_[:, 1, :], t3[:, :], t4[:, :], ALU.add, [bt3, bt4], [bhh])
                        if last_tile:
                            self.tt(S5O[:, 0, k:k + 1], t1[:, N - 1:N], t2[:, N - 1:N], ALU.subtract, [bt1, bt2], [bS5O])
                            self.tt(S5O[:, 1, k:k + 1], t3[:, N - 1:N], t4[:, N - 1:N], ALU.add, [bt3, bt4], [bS5O])
                        self.mm(pyc[:, 0:N], bct[:, kk, 2, :], hh_[:, 0, :], kk == 0, False, [bbct, bhh], [bpyc])
                        self.mm(pyc[:, 0:N], bct[:, kk, 3, :], hh_[:, 1, :], False, kk == 3, [bbct, bhh], [bpyc])
                        if kk == 3:
                            post(jc)
                        del S_[k]
                    a_mm(0)
                    a_dve(0)
                    a_pool(0)
                    a_scan(0)
                    yield
                    for k in range(16):
                        b_pool(k)
                        if k + 1 < 16:
                            a_mm(k + 1)
                            a_dve(k + 1)
                            a_pool(k + 1)
                        yield
                        b_dve(k)
                        if k + 1 < 16:
                            a_scan(k + 1)
                        yield
                for jc in range(4):
                    pt, bpt = pdense()
                    for kc in range(4):
                        self.mm(pt[:, 0:N], WGL[:, kc, jc * 128:(jc + 1) * 128], YGb[:, kc, :], kc == 0, kc == 3, [bWGL, bYGb], [bpt])
                    sg, bsg = tnext()
                    self.act(sg[:, :], pt[:, 0:N], AF.Sigmoid, [bpt], [bsg])
                    self.tt(sg[:, :], sg[:, :], YS[:, jc, :], ALU.mult, [bsg, bYS2], [bsg])
                    self.tt(ACTB[:, 4 + jc, 0:N], sg[:, :], G2[:, jc, :], ALU.mult, [bsg, bG2], [bACT])
                    yield
            def rr(gs_, until=None):
                alive = list(gs_)
                while alive and (until is None or until in alive):
                    for g_ in list(alive):
                        try:
                            next(g_)
                        except StopIteration:
                            alive.remove(g_)
                return alive
            if smp:
                self.pd_banks = list(range(8))
                rr([g_proj([0, 1, 2, 3, 4])])
                self.pd_banks = [0, 1]
                rr([g_rwkv()])
                rr([g_s5()])
            else:
                self.pd_banks = list(range(8))
                gp = g_proj([0, 1, 2, 3, 4])
                for _ in range(15):
                    next(gp)
                self.pd_banks = [0, 1, 5, 6, 7]
                gr = g_rwkv()
                rest = rr([gr, gp], until=gp)
                self.pd_banks = [0, 1]
                rr(rest + [g_s5()])
            self.pd_banks = list(range(8))
            for half in range(2):
                wt, bw = wload_t(s_wout[l, half], 512, b_swout[l])
                for cc in range(4):
                    pt, bpt = pdense()
                    for kc in range(8):
                        self.mm(pt[:, 0:N], wt[:, kc, cc * 128:(cc + 1) * 128], ACTB[:, kc, 0:N], kc == 0, kc == 7, [bw, bACT], [bpt])
                    j = half * 4 + cc
                    self.tt(xT[:, j, 0:N], xT[:, j, 0:N], pt[:, 0:N], ALU.add, [bxT, bpt], [bxT])
            rmsnorm("norm_x", l, N)
            if not smp:
                P.dma("sp", KTl[:].rearrange("p a m -> p (a m)"), s_kt[l], reads=[b_skt[l]], writes=[bKTl])
                P.dma("sp", VMl[:].rearrange("p a m -> p (a m)"), s_vm[l], reads=[b_svm[l]], writes=[bVMl])
            for half in range(2):
                wt, bw = wload_t(s_wq[l, half], 512, b_swq[l])
                for cc in range(4):
                    pt, bpt = pdense()
                    for kc in range(8):
                        self.mm(pt[:, 0:N], wt[:, kc, cc * 128:(cc + 1) * 128], ACTB[:, kc, 0:N], kc == 0, kc == 7, [bw, bACT], [bpt])
                    self.cp(SCRB[:, half * 4 + cc, 0:N], pt[:, 0:N], [bpt], [bSCR], eng="act")
            if smp:
                smp_attn(l)
            if not smp:
                ets = {}

                def at_scores(h):
                    et, bet = ETb.next()
                    ets[h] = (et, bet)
                    for mc in range(2):
                        bk = (2 + mc) if h % 2 == 0 else mc
                        pt, bpt = PS[bk], BPS[bk]
                        for dc in range(2):
                            self.mm(pt[:, 0:N], KTl[:, 2 * h + dc, mc * 128:(mc + 1) * 128], SCRB[:, 2 * h + dc, 0:N], dc == 0, dc == 1, [bKTl, bSCR], [bpt])
                        self.act(et[:, mc, :], pt[:, 0:N], AF.Exp, [bpt], [bet], scale=1.0 / 16.0)

                def at_pv(h):
                    et, bet = ets.pop(h)
                    hf = (h % 2) * 256
                    pdn, bpdn = PS[4][:, hf:hf + 256], BPS[4]
                    for mc in range(2):
                        self.mm(pdn[:, 0:N], onesb[:], et[:, mc, :], mc == 0, mc == 1, [bet, bc], [bpdn])
                    rd, brd = rden.next()
                    self.act(rd[:, :], pdn[:, 0:N], AF.Ln, [bpdn], [brd])
                    self.act(rd[:, :], rd[:, :], AF.Exp, [brd], [brd], scale=-1.0)
                    pob, bpo = PS[5 + h % 2], BPS[5 + h % 2]
                    for dc in range(2):
                        po = pob[:, dc * 256:dc * 256 + 256]
                        for mc in range(2):
                            self.mm(po[:, 0:N], VMl[:, mc, (2 * h + dc) * 128:(2 * h + dc + 1) * 128], et[:, mc, :], mc == 0, mc == 1, [bVMl, bet], [bpo])
                    for dc in range(2):
                        po = pob[:, dc * 256:dc * 256 + 256]
                        self.tt(ACTB[:, 2 * h + dc, 0:N], po[:, 0:N], rd[:, :], ALU.mult, [bpo, brd], [bACT])
                at_scores(0)
                for h in range(4):
                    if h + 1 < 4:
                        at_scores(h + 1)
                    at_pv(h)
            for half in range(2):
                wt, bw = wload_t(s_wo[l, half], 512, b_swo[l])
                for cc in range(4):
                    pt, bpt = pdense()
                    for kc in range(8):
                        self.mm(pt[:, 0:N], wt[:, kc, cc * 128:(cc + 1) * 128], ACTB[:, kc, 0:N], kc == 0, kc == 7, [bw, bACT], [bpt])
                    j = half * 4 + cc
                    self.tt(xT[:, j, 0:N], xT[:, j, 0:N], pt[:, 0:N], ALU.add, [bxT, bpt], [bxT])
            if smp:
                smp_store_shift(l)
            if last_tile:
                pt, bpt = PS[2], BPS[2]
                self.tr(pt[0:13, 0:128], carry[:, l, :], identf[:], [bcarry, bc], [bpt])
                self.cp(shs[0:13, :], pt[0:13, 0:128], [bpt], [bshs], eng="act")
                P.dma("sp", o_pshift[l].rearrange("(c p) -> c p", p=128), shs[0:13, :], reads=[bshs], final=True)
                pt, bpt = PS[3], BPS[3]
                for c in range(4):
                    self.tr(pt[0:64, c * 128:(c + 1) * 128], Hst[:, l, c, :], identf[:], [bH, bc], [bpt])
                self.cp(wko[:, :, :], pt[0:64, :].rearrange("p (c m) -> p c m", c=4), [bpt], [bwko], eng="act")
                P.dma("sp", o_pwkv[l].rearrange("(c h2) i j -> i c h2 j", h2=2), wko[:, :, :].rearrange("p c (h2 j) -> p c h2 j", h2=2), reads=[bwko], final=True)
                pt, bpt = PS[2], BPS[2]
                for ri in range(2):
                    self.tr(pt[0:16, ri * 128:(ri + 1) * 128], S5O[:, ri, :], identf[:], [bS5O, bc], [bpt])
                self.cp(s5o2[:, :, :], pt[0:16, 0:256].rearrange("p (a m) -> p a m", a=2), [bpt], [bs5o2], eng="act")
                P.dma("sp", o_ps5re[l].rearrange("(k g2) n -> k (g2 n)", g2=2), s5o2[:, 0, :], reads=[bs5o2], final=True)
                P.dma("sp", o_ps5im[l].rearrange("(k g2) n -> k (g2 n)", g2=2), s5o2[:, 1, :], reads=[bs5o2], final=True)

        for ti in range(NT):
            load_x(ti * N)
            if self.cfg.get("stop") != "C":
                for l in range(NL):
                    layer(l, ti == NT - 1, TN, False)
            store_y(ti * N)
        if not self.sample:
            P.emit()
            return nc
        P.emit(final=False)
        P.release(prompt_mark)
        P.barrier()
        xs = self.din("xs", [NB_S, T_S, D])
        st_shift = self.din("st_shift", [DEPTH, NB_S, SHIFT])
        st_wkv = self.din("st_wkv", [DEPTH, NB_S, 8, 64, 64])
        st_s5re = self.din("st_s5re", [DEPTH, NB_S, 2048])
        st_s5im = self.din("st_s5im", [DEPTH, NB_S, 2048])
        ck = self.din("ck", [DEPTH, NB_S, MEM, D])
        cv = self.din("cv", [DEPTH, NB_S, MEM, D])
        ys = self.dout("y_s", [NB_S, T_S, D])
        o_sshift = self.dout("s_shift", [DEPTH, NB_S, SHIFT])
        o_swkv = self.dout("s_wkv", [DEPTH, NB_S, 8, 64, 64])
        o_ss5re = self.dout("s_s5_re", [DEPTH, NB_S, 2048])
        o_ss5im = self.dout("s_s5_im", [DEPTH, NB_S, 2048])
        s_b1 = self.dscr("s_b1", [6, 64, 512])
        s_b2 = self.dscr("s_b2", [64, 512])
        bsb1, bsb2 = Buf(), Buf()
        SSH = P.sb("SSH", [128, 13, 16]); bSSH = Buf()
        NSH = P.sb("NSH", [128, 13, 16]); bNSH = Buf()
        TMS = Rot(P, "TMS", 2, [64, 6, 128])
        X6 = P.sb("X6", [128, 6, 4, 64]); bX6s = [Buf() for _ in range(3)]
        Ssm = P.sb("Ssm", [128, 64, 64]); bSlo, bShi = Buf(), Buf()
        T1s = P.sb("T1s", [128, 64, 64]); bT1lo, bT1hi = Buf(), Buf()
        sks = P.sb("sks", [128, 64]); bsk = Buf()
        Ysm = P.sb("Ysm", [128, 4, 64]); bYsm = Buf()
        ytm = P.sb("ytm", [64, 512]); bytm = Buf()
        Hs5 = P.sb("Hs5", [128, 2, 2, 16, 16]); bHs5 = Buf()
        HsT = P.sb("HsT", [128, 2, 16, 64], BF16); bHsT = Buf()
        s5t = P.sb("s5t", [128, 2, 16, 16]); bs5t = Buf()
        Kf32 = P.sb("Kf32", [128, 2, D]); bKf32 = Buf()
        KTs = P.sb("KTs", [128, 8, MEM], BF16); bKTs = Buf()
        Vs = P.sb("Vs", [128, 2, D], BF16); bVs = Buf()
        ETs = P.sb("ETs", [128, 16, 32], BF16); bETs = Buf()
        rds = P.sb("rds", [128, 16, 16]); brds = Buf()
        print("SBUF bytes/partition (sample phase):", P.sb_bytes)
        NS = NB_S * T_S
        xi, bxi = xin.next()
        for t in range(T_S):
            P.dma("sp", xi[t * 16:(t + 1) * 16, :], xs[:, t, :], writes=[bxi])
        for half in range(2):
            pt, bpt = PS[2 + half], BPS[2 + half]
            for q in range(4):
                kc = half * 4 + q
                self.tr(pt[:, q * 64:(q + 1) * 64], xi[0:64, kc * 128:(kc + 1) * 128], identf[0:64, 0:64], [bxi, bc], [bpt])
            self.cp(xT[:, half * 4:(half + 1) * 4, 0:NS], pt[:, 0:256].rearrange("p (q m) -> p q m", q=4), [bpt], [bxT], eng="act")
        for l in range(NL):
            layer(l, False, NS, True)
        self.act(SCRB[:, :, 0:NS], xT[:, :, 0:NS], AF.Square, [bxT], [bSCR])
        pt, bpt = pdense()
        for kc in range(8):
            self.mm(pt[:, 0:NS], onesb[:], SCRB[:, kc, 0:NS], kc == 0, kc == 7, [bSCR, bc], [bpt])
        self.act(rstd[:, 0:NS], pt[:, 0:NS], AF.Ln, [bpt], [brstd], bias=NORM_EPS, scale=1.0 / D)
        self.act(rstd[:, 0:NS], rstd[:, 0:NS], AF.Exp, [brstd], [brstd], scale=-0.5)
        for kc in range(8):
            self.stt(xT[:, kc, 0:NS], xT[:, kc, 0:NS], pc("norm_f", 0, kc), rstd[:, 0:NS], ALU.mult, ALU.mult, [bxT, brstd, bprm], [bxT])
        xi, bxi = xin.next()
        for half in range(2):
            pt, bpt = PS[2 + half], BPS[2 + half]
            for q in range(4):
                kc = half * 4 + q
                self.tr(pt[0:64, q * 128:(q + 1) * 128], xT[:, kc, 0:NS], identf[:], [bxT, bc], [bpt])
            self.cp(xi[0:64, half * 512:(half + 1) * 512], pt[0:64, :], [bpt], [bxi], eng="act")
        for t in range(T_S):
            P.dma("sp", ys[:, t, :], xi[t * 16:(t + 1) * 16, :], reads=[bxi], final=True)
        print("dma semaphores used:", P.nsem)
        P.emit()
        return nc


def _core_inputs(inp, core, cfg):
    TN = cfg.get("TN", 256)
    NT = cfg.get("NT", SEQ // TN)
    m = {}
    m["xp"] = np.ascontiguousarray(inp["x_prompt"][core, :NT * TN])
    m["memp"] = np.ascontiguousarray(inp["mem_prompt"][core])
    for n in ("w_in", "w_out", "wq", "wk", "wv", "wo", "w_glu", "w2", "a2", "v1", "v2", "norm_mix", "norm_x", "norm_mem",
              "mu_shift", "w0", "a0", "k_k", "k_a", "gn_w", "gn_b", "d_skip", "v0", "log_dt", "b_re", "b_im", "c_re", "c_im"):
        m[n] = inp[n]
    m["norm_f"] = inp["norm_f"].reshape(1, D)
    m["r_k"] = inp["r_k"].reshape(DEPTH, RW)
    m["lam_re"] = inp["lam_re"].reshape(DEPTH, 2048)
    m["lam_im"] = inp["lam_im"].reshape(DEPTH, 2048)
    if cfg.get("sample", True):
        bs = slice(core * NB_S, (core + 1) * NB_S)
        m["xs"] = np.ascontiguousarray(inp["x_sample"][bs])
        m["st_shift"] = np.ascontiguousarray(inp["state_shift"][:, bs])
        m["st_wkv"] = np.ascontiguousarray(inp["state_wkv"][:, bs])
        m["st_s5re"] = np.ascontiguousarray(inp["state_s5_re"][:, bs]).reshape(DEPTH, NB_S, 2048)
        m["st_s5im"] = np.ascontiguousarray(inp["state_s5_im"][:, bs]).reshape(DEPTH, NB_S, 2048)
        m["ck"] = np.ascontiguousarray(inp["cache_mem_k"][:, bs]).reshape(DEPTH, NB_S, MEM, D)
        m["cv"] = np.ascontiguousarray(inp["cache_mem_v"][:, bs]).reshape(DEPTH, NB_S, MEM, D)
    return m


_NC_CACHE = {}


def run(inp, cfg, cores=8):
    key = tuple(sorted(cfg.items()))
    if key not in _NC_CACHE:
        _NC_CACHE[key] = K(cfg).build()
    nc = _NC_CACHE[key]
    inp = {n: np.asarray(v) for n, v in inp.items()}
    in_maps = [_core_inputs(inp, c, cfg) for c in range(cores)]
    res = run_bass_kernel_spmd(nc, in_maps, core_ids=list(range(cores)))
    return res.results


def kernel(**inputs):
    cfg = {}
    res = run(inputs, cfg, 8)
    f = np.float32
    cat = lambda k: np.stack([np.asarray(r[k], dtype=f) for r in res], axis=0)
    y_prompt = cat("y_p")
    y_sample = cat("y_s").reshape(8 * NB_S, T_S, D)
    st1 = lambda k, shp: np.ascontiguousarray(np.moveaxis(cat(k), 0, 1)).reshape(shp)
    p_shift = st1("p_shift", (DEPTH, 8, SHIFT))
    p_wkv = st1("p_wkv", (DEPTH, 8, 8, 64, 64))
    p_s5_re = st1("p_s5_re", (DEPTH, 8, 32, 64))
    p_s5_im = st1("p_s5_im", (DEPTH, 8, 32, 64))
    p_mem_k = st1("p_mem_k", (DEPTH, 8, MEM, 4, 256))
    p_mem_v = st1("p_mem_v", (DEPTH, 8, MEM, 4, 256))
    s_shift = st1("s_shift", (DEPTH, 8 * NB_S, SHIFT))
    s_wkv = st1("s_wkv", (DEPTH, 8 * NB_S, 8, 64, 64))
    s_s5_re = st1("s_s5_re", (DEPTH, 8 * NB_S, 32, 64))
    s_s5_im = st1("s_s5_im", (DEPTH, 8 * NB_S, 32, 64))
    return (y_prompt, y_sample, p_shift, p_wkv, p_s5_re, p_s5_im, p_mem_k, p_mem_v, s_shift, s_wkv, s_s5_re, s_s5_im)
```

```python
import math
import numpy as np
import concourse.bass as bass
import concourse.mybir as mybir
from concourse.bass_utils import run_bass_kernel_spmd

F32 = mybir.dt.float32
BF16 = mybir.dt.bfloat16
I32 = mybir.dt.int32
AF = mybir.ActivationFunctionType
ALU = mybir.AluOpType

D = 1024
DEPTH = 4
SEQ = 2048
NB_S = 16
T_S = 4
RW = 512
SHIFT = 1664
INC = 3200
MEM = 256
C0 = math.exp(-0.5)
NORM_EPS = 1e-6
GN_EPS = 64e-5
TWO_PI = 2.0 * math.pi
CW1 = 6.28125
CW2 = TWO_PI - CW1


class Buf:
    __slots__ = ("w", "r", "sem", "semv", "excl")

    def __init__(self, excl=False):
        self.w = None
        self.r = {}
        self.sem = None
        self.semv = 0
        self.excl = excl


class Prog:
    ENG = ("pe", "act", "dve", "pool", "sp")

    def __init__(self, nc):
        self.nc = nc
        self.ops = {e: [] for e in self.ENG}
        self.cnt = {e: 0 for e in self.ENG}
        self.sems = {}
        self.seen = {e: {} for e in self.ENG}
        self.nsem = 0
        for e in self.ENG:
            self.sems[("eng", e)] = nc.alloc_semaphore(name="prog_" + e)
        self.out_events = {}
        self.ctx = []
        self.ctxb = []
        self.sb_bytes = 0
        self.dmav = {}
        self.pending = {e: {} for e in self.ENG}

    def sb(self, name, shape, dtype=F32):
        g = self.nc.sbuf_tensor(name, list(shape), dtype)
        t = g.__enter__()
        self.ctx.append(g)
        n = 1
        for s in shape[1:]:
            n *= s
        nb = n * (4 if dtype in (F32, I32) else 2)
        self.sb_bytes += nb
        self.ctxb.append(nb)
        return t

    def mark(self):
        return len(self.ctx)

    def release(self, mark):
        while len(self.ctx) > mark:
            self.ctx.pop().__exit__(None, None, None)
            self.sb_bytes -= self.ctxb.pop()

    def barrier(self):
        cur = {("eng", e): self.cnt[e] for e in self.ENG}
        cur.update(self.dmav)
        for e in self.ENG:
            for k, v in cur.items():
                if v > 0 and self.pending[e].get(k, 0) < v:
                    self.pending[e][k] = v

    def ps(self, name, shape, dtype=F32):
        g = self.nc.psum_tensor(name, list(shape), dtype)
        t = g.__enter__()
        self.ctx.append(g)
        self.ctxb.append(0)
        return t

    def _dsem(self, b):
        if b.sem is None:
            self.nsem += 1
            key = ("dma", self.nsem)
            self.sems[key] = self.nc.alloc_semaphore(name="dq%d" % self.nsem)
            b.sem = key
        return b.sem

    def pe_fence(self):
        if self.cnt["pe"] > 0:
            self.pending["pe"][("eng", "pe")] = self.cnt["pe"]

    def _waits(self, eng, reads, writes):
        need = {}

        def add(ev):
            if ev is None:
                return
            k, v = ev
            if need.get(k, 0) < v:
                need[k] = v
        for b in reads:
            add(b.w)
        for b in writes:
            add(b.w)
            for k, v in b.r.items():
                add((k, v))
        own = ("eng", eng)
        if eng == "pe":
            need.pop(own, None)
        if self.pending[eng]:
            for k, v in self.pending[eng].items():
                add((k, v))
            self.pending[eng] = {}
        out = []
        seen = self.seen[eng]
        for k, v in need.items():
            if seen.get(k, 0) < v:
                seen[k] = v
                out.append((k, v))
        return out

    def _commit(self, ev, reads, writes):
        k, v = ev
        for b in reads:
            if b.r.get(k, 0) < v:
                b.r[k] = v
        for b in writes:
            b.w = ev
            b.r = {}

    def op(self, eng, fn, reads=(), writes=()):
        if any(b.excl for b in reads):
            writes = list(writes) + [b for b in reads if b.excl]
            reads = [b for b in reads if not b.excl]
        waits = self._waits(eng, reads, writes)
        self.cnt[eng] += 1
        ev = (("eng", eng), self.cnt[eng])
        self.ops[eng].append((waits, fn, (ev[0], 1)))
        self._commit(ev, reads, writes)
        return ev

    def dma(self, eng, out, in_, reads=(), writes=(), sembuf=None, final=False, **kw):
        if sembuf is None:
            sembuf = writes[0] if writes else reads[0]
        waits = self._waits(eng, reads, writes)
        key = self._dsem(sembuf)
        sembuf.semv += 16
        ev = (key, sembuf.semv)
        self.dmav[key] = sembuf.semv

        def fn(e, out=out, in_=in_, kw=kw):
            return e.dma_start(out=out, in_=in_, **kw)
        self.ops[eng].append((waits, fn, (key, 16)))
        self._commit(ev, reads, writes)
        if final and self.out_events.get(key, 0) < ev[1]:
            self.out_events[key] = ev[1]
        return ev

    def emit(self, final=True):
        nc = self.nc
        fin = list(self.out_events.items()) if final else []
        hmap = {"pe": "tensor", "act": "scalar", "dve": "vector", "pool": "gpsimd", "sp": "sync"}
        with nc.Block() as block:
            for eng in self.ENG:
                ops = self.ops[eng]
                extra = fin if eng == "sp" else []

                def body(e, ops=ops, extra=extra):
                    for waits, fn, inc in ops:
                        for k, v in waits:
                            e.wait_ge(self.sems[k], v)
                        ins = fn(e)
                        ins.then_inc(self.sems[inc[0]], inc[1])
                    for k, v in extra:
                        e.wait_ge(self.sems[k], v)
                getattr(block, hmap[eng])(body)
        self.ops = {e: [] for e in self.ENG}
        if final:
            self.release(0)


class Rot:
    def __init__(self, P, name, n, shape, dtype=F32, psum=False):
        self.t = [(P.ps if psum else P.sb)("%s%d" % (name, i), shape, dtype) for i in range(n)]
        self.b = [Buf() for _ in range(n)]
        self.i = 0

    def next(self):
        i = self.i
        self.i = (i + 1) % len(self.t)
        return self.t[i], self.b[i]


class K:
    def __init__(self, cfg):
        self.cfg = cfg
        self.TN = cfg.get("TN", 256)
        self.NT = cfg.get("NT", SEQ // self.TN)
        self.NL = cfg.get("NL", DEPTH)
        self.sample = cfg.get("sample", True)
        self.nc = bass.Bass("TRN2", target_bir_lowering=False)
        self.P = Prog(self.nc)
        self.dram = {}

    def din(self, name, shape, dt=F32):
        a = self.nc.dram_tensor(name, list(shape), dt, kind="ExternalInput").ap()
        self.dram[name] = a
        return a

    def dout(self, name, shape, dt=F32):
        a = self.nc.dram_tensor(name, list(shape), dt, kind="ExternalOutput").ap()
        self.dram[name] = a
        return a

    def dscr(self, name, shape, dt=F32):
        a = self.nc.dram_tensor(name, list(shape), dt, kind="Internal").ap()
        self.dram[name] = a
        return a

    def mm(self, out, lhsT, rhs, start, stop, r, w):
        return self.P.op("pe", lambda e: e.matmul(out, lhsT=lhsT, rhs=rhs, start=start, stop=stop), reads=r, writes=w)

    def tr(self, out, in_, ident, r, w):
        return self.P.op("pe", lambda e: e.transpose(out=out, in_=in_, identity=ident), reads=r, writes=w)

    def act(self, out, in_, func, r, w, bias=None, scale=None, eng="act"):
        kw = {}
        if bias is not None:
            kw["bias"] = bias
        if scale is not None:
            kw["scale"] = scale
        return self.P.op(eng, lambda e: e.activation(out=out, in_=in_, func=func, **kw), reads=r, writes=w)

    def tt(self, out, in0, in1, op, r, w, eng="dve"):
        return self.P.op(eng, lambda e: e.tensor_tensor(out=out, in0=in0, in1=in1, op=op), reads=r, writes=w)

    def ts(self, out, in0, s1, op0, r, w, s2=None, op1=None, eng="dve"):
        if op1 is None:
            return self.P.op(eng, lambda e: e.tensor_scalar(out=out, in0=in0, scalar1=s1, scalar2=None, op0=op0), reads=r, writes=w)
        return self.P.op(eng, lambda e: e.tensor_scalar(out=out, in0=in0, scalar1=s1, scalar2=s2, op0=op0, op1=op1), reads=r, writes=w)

    def stt(self, out, in0, scalar, in1, op0, op1, r, w):
        return self.P.op("dve", lambda e: e.scalar_tensor_tensor(out=out, in0=in0, scalar=scalar, in1=in1, op0=op0, op1=op1), reads=r, writes=w)

    def cp(self, out, in_, r, w, eng="dve"):
        if eng == "act":
            return self.act(out, in_, AF.Copy, r, w)
        return self.P.op(eng, lambda e: e.tensor_copy(out=out, in_=in_), reads=r, writes=w)

    def recip(self, out, in_, r, w):
        return self.P.op("dve", lambda e: e.reciprocal(out=out, in_=in_), reads=r, writes=w)

    def recipf(self, out, in_, r, w):
        return self.P.op("dve", lambda e: e.reciprocal_approx_fast(out=out, in_=in_), reads=r, writes=w)

    def scan(self, out, d0, d1, init, r, w):
        return self.P.op("dve", lambda e: e.tensor_tensor_scan(out=out, data0=d0, data1=d1, initial=init, op0=ALU.mult, op1=ALU.add), reads=r, writes=w)

    def memset(self, ap, val, w, eng="pool"):
        return self.P.op(eng, lambda e: e.memset(ap, val), writes=w)

    def build(self):
        P, nc = self.P, self.nc
        TN, NT, NL = self.TN, self.NT, self.NL
        SQ = NT * TN
        xp = self.din("xp", [SQ, D])
        memp = self.din("memp", [MEM, D])
        win = self.din("w_in", [DEPTH, D, INC])
        wout = self.din("w_out", [DEPTH, D, D])
        wq = self.din("wq", [DEPTH, D, D])
        wk = self.din("wk", [DEPTH, D, D])
        wv = self.din("wv", [DEPTH, D, D])
        wo = self.din("wo", [DEPTH, D, D])
        wglu = self.din("w_glu", [DEPTH, RW, RW])
        w2 = self.din("w2", [DEPTH, 64, RW])
        a2 = self.din("a2", [DEPTH, 64, RW])
        v1 = self.din("v1", [DEPTH - 1, RW, 32])
        v2 = self.din("v2", [DEPTH - 1, 32, RW])
        prm = {}
        for nm, wd, L in (("norm_mix", D, DEPTH), ("norm_x", D, DEPTH), ("norm_mem", D, DEPTH), ("norm_f", D, 1),
                          ("mu_shift", SHIFT, DEPTH), ("w0", RW, DEPTH), ("a0", RW, DEPTH), ("k_k", RW, DEPTH),
                          ("k_a", RW, DEPTH), ("gn_w", RW, DEPTH), ("gn_b", RW, DEPTH), ("r_k", RW, DEPTH),
                          ("d_skip", RW, DEPTH), ("v0", RW, DEPTH - 1), ("lam_re", 2048, DEPTH), ("lam_im", 2048, DEPTH)):
            prm[nm] = (self.din(nm, [L, wd]), wd // 128, L)
        logdt = self.din("log_dt", [DEPTH, 32])
        bre_d = self.din("b_re", [DEPTH, 32, 64, 16])
        bim_d = self.din("b_im", [DEPTH, 32, 64, 16])
        cre_d = self.din("c_re", [DEPTH, 32, 16, 64])
        cim_d = self.din("c_im", [DEPTH, 32, 16, 64])

        yp = self.dout("y_p", [SQ, D])
        o_pshift = self.dout("p_shift", [DEPTH, SHIFT])
        o_pwkv = self.dout("p_wkv", [DEPTH, 8, 64, 64])
        o_ps5re = self.dout("p_s5_re", [DEPTH, 32, 64])
        o_ps5im = self.dout("p_s5_im", [DEPTH, 32, 64])
        o_pmk = self.dout("p_mem_k", [DEPTH, MEM, D])
        o_pmv = self.dout("p_mem_v", [DEPTH, MEM, D])

        s_win = self.dscr("s_win", [DEPTH, 5, 128, 8 * 640], BF16)
        s_wout = self.dscr("s_wout", [DEPTH, 2, 128, 8 * 512], BF16)
        s_wq = self.dscr("s_wq", [DEPTH, 2, 128, 8 * 512], BF16)
        s_wo = self.dscr("s_wo", [DEPTH, 2, 128, 8 * 512], BF16)
        s_kt = self.dscr("s_kt", [DEPTH, 128, 8 * MEM], BF16)
        s_vm = self.dscr("s_vm", [DEPTH, 128, 2 * D], BF16)
        s_bc = self.dscr("s_bc", [DEPTH, 128, 4, 4 * 4 * 128], BF16)
        s_cs = self.dscr("s_cs", [DEPTH, 128, 2 * 16 * 129], F32)
        b_swin = [Buf() for _ in range(DEPTH)]
        b_swout = [Buf() for _ in range(DEPTH)]
        b_swq = [Buf() for _ in range(DEPTH)]
        b_swo = [Buf() for _ in range(DEPTH)]
        b_skt = [Buf() for _ in range(DEPTH)]
        b_svm = [Buf() for _ in range(DEPTH)]
        b_sbc = [Buf() for _ in range(DEPTH)]
        b_scs = [Buf() for _ in range(DEPTH)]

        PS = [P.ps("psb%d" % i, [128, 512]) for i in range(8)]
        BPS = [Buf(excl=True) for _ in range(8)]
        self.pd_i = 0

        self.pd_banks = [0, 1]

        def pdense():
            bk = self.pd_banks
            self.pd_i = (self.pd_i + 1) % len(bk)
            i = bk[self.pd_i]
            return PS[i], BPS[i]

        identf = P.sb("identf", [128, 128])
        identb = P.sb("identb", [128, 128], BF16)
        onesf = P.sb("onesf", [128, 128])
        bonesf = P.sb("bonesf", [128, 128])
        bonesb = P.sb("bonesb", [128, 128], BF16)
        onesb = P.sb("onesb", [128, 128], BF16)
        mska = P.sb("mska", [128, 128])
        mskl = P.sb("mskl", [128, 64])
        cmask = P.sb("cmask", [128, TN])
        tau = P.sb("tau", [128, 129])
        bc = Buf()
        self.memset(identf[:], 0.0, [bc])
        P.op("pool", lambda e: e.affine_select(out=identf[:], in_=identf[:], pattern=[[-1, 128]], compare_op=ALU.not_equal, fill=1.0, base=0, channel_multiplier=1), reads=[bc], writes=[bc])
        self.cp(identb[:], identf[:], [bc], [bc], eng="pool")
        self.memset(onesf[:], 1.0, [bc])
        self.memset(onesb[:], 1.0, [bc])
        self.memset(bonesf[:], 0.0, [bc])
        self.memset(bonesf[0:64, 0:64], 1.0 / 64.0, [bc])
        self.memset(bonesf[64:128, 64:128], 1.0 / 64.0, [bc])
        self.memset(bonesb[:], 0.0, [bc])
        self.memset(bonesb[0:64, 0:64], 1.0, [bc])
        self.memset(bonesb[64:128, 64:128], 1.0, [bc])
        self.memset(mska[:], 1.0, [bc])
        for hb in (0, 64):
            P.op("pool", lambda e, hb=hb: e.affine_select(out=mska[hb:hb + 64, 0:64], in_=mska[hb:hb + 64, 0:64], pattern=[[1, 64]], compare_op=ALU.is_gt, fill=0.0, base=0, channel_multiplier=-1), reads=[bc], writes=[bc])
            P.op("pool", lambda e, hb=hb: e.affine_select(out=mska[hb:hb + 64, 64:128], in_=mska[hb:hb + 64, 64:128], pattern=[[1, 64]], compare_op=ALU.is_ge, fill=0.0, base=0, channel_multiplier=-1), reads=[bc], writes=[bc])
        self.memset(mskl[:], 1.0, [bc])
        for hb in (0, 64):
            P.op("pool", lambda e, hb=hb: e.affine_select(out=mskl[hb:hb + 64, :], in_=mskl[hb:hb + 64, :], pattern=[[-1, 64]], compare_op=ALU.is_gt, fill=0.0, base=0, channel_multiplier=1), reads=[bc], writes=[bc])
        self.memset(cmask[:], 1.0, [bc])
        self.memset(cmask[:].rearrange("p (a b) -> p a b", b=64)[:, :, 0:1], 0.0, [bc])
        P.op("pool", lambda e: e.iota(tau[:], pattern=[[1, 129]], base=0, channel_multiplier=0, allow_small_or_imprecise_dtypes=True), writes=[bc])
        self.identf, self.identb, self.bc = identf, identb, bc

        if self.cfg.get("stop") == "const":
            dbg = self.dout("dbg", [128, 128])
            P.dma("sp", dbg[:, :], mska[:], reads=[bc], final=True)
            P.emit()
            return nc
        order = [["norm_mix", "norm_x", "norm_mem", "norm_f"],
                 ["mu_shift", "w0", "a0", "k_k", "k_a"],
                 ["gn_w", "gn_b", "r_k", "d_skip", "v0"],
                 ["lam_re", "lam_im"]]
        ncols = sum(prm[n][1] * prm[n][2] for g in order for n in g)
        PRM = P.sb("PRM", [128, ncols + 64])
        bprm = Buf()
        col = {}
        c0 = 0
        for gi, g in enumerate(order):
            rows = sum(prm[n][1] * prm[n][2] for n in g)
            stg = P.sb("stg%d" % gi, [128, 128])
            bst = Buf()
            r0 = 0
            for n in g:
                ap, nch, L = prm[n]
                nr = nch * L
                P.dma("sp", stg[r0:r0 + nr, :], ap.rearrange("l (c p) -> (l c) p", p=128), writes=[bst])
                col[n] = (c0 + r0, nch)
                r0 += nr
            pt, bpt = PS[2 + gi % 2], BPS[2 + gi % 2]
            self.tr(pt[:, 0:rows], stg[0:rows, :], identf[0:rows, 0:rows], [bst, bc], [bpt])
            self.cp(PRM[:, c0:c0 + rows], pt[:, 0:rows], [bpt], [bprm], eng="act")
            c0 += rows
        self.PRM, self.bprm, self.col = PRM, bprm, col

        def pc(name, l, c):
            c00, nch = col[name]
            return PRM[:, c00 + l * nch + c: c00 + l * nch + c + 1]
        self.pc = pc
        OMM = P.sb("OMM", [128, DEPTH * 13])
        OMK = P.sb("OMK", [128, DEPTH * 4])
        cm, _ = col["mu_shift"]
        ck, _ = col["k_a"]
        self.ts(OMM[:], PRM[:, cm:cm + DEPTH * 13], -1.0, ALU.mult, [bprm], [bprm], s2=1.0, op1=ALU.add)
        self.ts(OMK[:], PRM[:, ck:ck + DEPTH * 4], -1.0, ALU.mult, [bprm], [bprm], s2=1.0, op1=ALU.add)

        if self.cfg.get("stop") == "prm":
            dbg = self.dout("dbg", [128, ncols])
            P.dma("sp", dbg[:, :], PRM[:, 0:ncols], reads=[bprm], final=True)
            P.emit()
            return nc
        def precast(l):
            for h in range(5):
                P.dma("pool", s_win[l, h].rearrange("p (kc c) -> p kc c", kc=8), win[l, :, h * 640:(h + 1) * 640].rearrange("(kc p) c -> p kc c", p=128), writes=[b_swin[l]])
            for (src, dst, bb) in ((wout, s_wout, b_swout), (wq, s_wq, b_swq), (wo, s_wo, b_swo)):
                for h in range(2):
                    P.dma("pool", dst[l, h].rearrange("p (kc c) -> p kc c", kc=8), src[l, :, h * 512:(h + 1) * 512].rearrange("(kc p) c -> p kc c", p=128), writes=[bb[l]])
        if self.cfg.get("stop") == "pre":
            precast(0)

        if self.cfg.get("stop") == "pre":
            dbgt = P.sb("dbgt", [128, 8, 640], BF16)
            bdbg = Buf()
            P.dma("sp", dbgt[:], s_win[0, 0].rearrange("p (kc c) -> p kc c", kc=8), reads=[b_swin[0]], writes=[bdbg])
            dbg = self.dout("dbg", [128, 8, 640], BF16)
            P.dma("sp", dbg[:, :, :], dbgt[:], reads=[bdbg], final=True)
            P.emit()
            return nc
        WB = Rot(P, "WB", 2, [128, 8, 640], BF16)

        def wload(src_l, c_lo, ncol, rbuf, eng="sp"):
            t, b = WB.next()
            P.dma(eng, t[:, :, 0:ncol], src_l[:, c_lo:c_lo + ncol].rearrange("(kc p) c -> p kc c", p=128), reads=[rbuf] if rbuf else [], writes=[b])
            return t, b

        def wload_t(src_lg, ncol, rbuf):
            t, b = WB.next()
            P.dma("sp", t[:, :, 0:ncol], src_lg.rearrange("p (kc c) -> p kc c", kc=8), reads=[rbuf], writes=[b])
            return t, b

        xT = P.sb("xT", [128, 8, TN])
        bxT = Buf()
        ACTB = P.sb("ACTB", [128, 8, TN], BF16)
        bACTk = [Buf() for _ in range(8)]
        SCRB = P.sb("SCRB", [128, 8, TN], BF16)
        bSCRk = [Buf() for _ in range(8)]
        rstd = P.sb("rstd", [128, TN])
        brstd = Buf()

        def rmsnorm(gname, l, N):
            self.act(SCRB[:, :, 0:N], xT[:, :, 0:N], AF.Square, [bxT], bSCRk)
            pt, bpt = pdense()
            for kc in range(8):
                self.mm(pt[:, 0:N], onesb[:], SCRB[:, kc, 0:N], kc == 0, kc == 7, [bSCRk[kc], bc], [bpt])
            self.act(rstd[:, 0:N], pt[:, 0:N], AF.Ln, [bpt], [brstd], bias=NORM_EPS, scale=1.0 / D)
            self.act(rstd[:, 0:N], rstd[:, 0:N], AF.Exp, [brstd], [brstd], scale=-0.5)
            for kc in range(8):
                self.stt(ACTB[:, kc, 0:N], xT[:, kc, 0:N], pc(gname, l, kc), rstd[:, 0:N], ALU.mult, ALU.mult, [bxT, brstd, bprm], [bACTk[kc]])

        S5P = P.sb("S5P", [128, 12, DEPTH * 16])
        setup_mark = P.mark()
        mtm = P.sb("mtm", [128, 2, D])
        bmtm = Buf()
        P.dma("sp", mtm[:], memp.rearrange("(a p) d -> p a d", p=128), writes=[bmtm])
        mss = P.sb("mss", [128, 2])
        bmss = Buf()
        mjunk = P.sb("mjunk", [128, D], BF16)
        for a in range(2):
            P.op("act", lambda e, a=a: e.activation(out=mjunk[:], in_=mtm[:, a, :], func=AF.Square, accum_out=mss[:, a:a + 1]), reads=[bmtm], writes=[bmss])
        self.act(mss[:], mss[:], AF.Sqrt, [bmss], [bmss], bias=NORM_EPS, scale=1.0 / D)
        self.recip(mss[:], mss[:], [bmss], [bmss])
        for a in range(2):
            self.ts(mtm[:, a, :], mtm[:, a, :], mss[:, a:a + 1], ALU.mult, [bmtm, bmss], [bmtm])
        mT = P.sb("mT", [128, 8, MEM])
        bmT = Buf()
        for a in range(2):
            for half in range(2):
                pt, bpt = PS[2 + half], BPS[2 + half]
                for q in range(4):
                    kc = half * 4 + q
                    self.tr(pt[:, q * 128:(q + 1) * 128], mtm[:, a, kc * 128:(kc + 1) * 128], identf[:], [bmtm, bc], [bpt])
                self.cp(mT[:, half * 4:(half + 1) * 4, a * 128:(a + 1) * 128], pt[:].rearrange("p (q m) -> p q m", q=4), [bpt], [bmT], eng="act")
        mnT = P.sb("mnT", [128, 8, MEM], BF16)
        bmnT = Buf()
        kts = P.sb("kts", [128, 8, MEM], BF16)
        bkts = Buf()
        vms = P.sb("vms", [128, 2, D], BF16)
        bvms = Buf()
        ostg = Rot(P, "ostg", 2, [128, 512])
        for l in range(NL):
            for kc in range(8):
                self.ts(mnT[:, kc, :], mT[:, kc, :], pc("norm_mem", l, kc), ALU.mult, [bmT, bprm], [bmnT])
            for which, wsrc in ((0, wk), (1, wv)):
                for half in range(2):
                    t, b = wload(wsrc[l], half * 512, 512, None, eng="pool")
                    if which == 0:
                        for cc in range(4):
                            pt, bpt = pdense()
                            for kc in range(8):
                                self.mm(pt[:, 0:MEM], t[:, kc, cc * 128:(cc + 1) * 128], mnT[:, kc, :], kc == 0, kc == 7, [b, bmnT], [bpt])
                            self.cp(kts[:, half * 4 + cc, :], pt[:, 0:MEM], [bpt], [bkts], eng="act")
                    for a in range(2):
                        pt, bpt = pdense()
                        for kc in range(8):
                            self.mm(pt[:, :], mnT[:, kc, a * 128:(a + 1) * 128], t[:, kc, 0:512], kc == 0, kc == 7, [b, bmnT], [bpt])
                        og, bog = ostg.next()
                        self.cp(og[:], pt[:], [bpt], [bog], eng="act")
                        if which == 1:
                            self.cp(vms[:, a, half * 512:(half + 1) * 512], pt[:], [bpt], [bvms], eng="dve")
                        dst = (o_pmk if which == 0 else o_pmv)[l, a * 128:(a + 1) * 128, half * 512:(half + 1) * 512]
                        P.dma("sp", dst, og[:], reads=[bog], final=True)
            P.dma("sp", s_kt[l], kts[:].rearrange("p a m -> p (a m)"), reads=[bkts], writes=[b_skt[l]])
            P.dma("sp", s_vm[l], vms[:].rearrange("p a m -> p (a m)"), reads=[bvms], writes=[b_svm[l]])

        precast(0)
        if self.cfg.get("stop") == "A":
            P.emit()
            return nc
        LD = P.sb("LD", [128, DEPTH, 16])
        bLD = Buf()
        for l in range(NL):
            for g2 in range(2):
                P.dma("sp", LD[g2 * 64:(g2 + 1) * 64, l, :], logdt[l:l + 1, g2::2].partition_broadcast(64), writes=[bLD], allow_slow_non_contiguous=True)
        clr, _ = col["lam_re"]
        cli, _ = col["lam_im"]
        NLK = NL * 16
        LR = PRM[:, clr:clr + NLK]
        LI = PRM[:, cli:cli + NLK]
        bS5P = Buf()
        DT, TH, RHO, LBR, LBI, FRE, FIM, T0, T1, T2, DEN, T3 = [S5P[:, i, 0:NLK] for i in range(12)]
        LDf = LD[:, 0:NL, :].rearrange("p l k -> p (l k)")
        r_, w_ = [bS5P, bprm, bLD], [bS5P]
        self.act(DT, LDf, AF.Exp, r_, w_)
        self.tt(TH, LI, DT, ALU.mult, r_, w_)
        self.tt(T0, LR, DT, ALU.mult, r_, w_)
        self.act(RHO, T0, AF.Exp, r_, w_)

        def sincos(out_sin, x, n, r, w, shift=0.0, tmpf=None, tmpi=None):
            self.ts(tmpf, x, 1.0, ALU.mult, r, w, s2=shift, op1=ALU.add)
            self.ts(tmpi, tmpf, 1.0 / TWO_PI, ALU.mult, r, w)
            self.cp(out_sin, tmpi, r, w)
            self.stt(tmpf, out_sin, -CW1, tmpf, ALU.mult, ALU.add, r, w)
            self.stt(tmpf, out_sin, -CW2, tmpf, ALU.mult, ALU.add, r, w)
            self.ts(tmpf, tmpf, math.pi, ALU.min, r, w, s2=-math.pi, op1=ALU.max)
            self.act(out_sin, tmpf, AF.Sin, r, w)
        S5I = P.sb("S5I", [128, 16 * 129], I32)
        S5F = P.sb("S5F", [128, 16 * 129])
        sincos(T1, TH, NLK, r_, w_, 0.0, T3, S5I[:, 0:NLK])
        sincos(T2, TH, NLK, r_, w_, math.pi / 2, T3, S5I[:, 0:NLK])
        self.tt(LBI, RHO, T1, ALU.mult, r_, w_)
        self.tt(LBR, RHO, T2, ALU.mult, r_, w_)
        self.ts(T0, LBR, -1.0, ALU.add, r_, w_)
        self.tt(T1, LR, LR, ALU.mult, r_, w_)
        self.tt(T2, LI, LI, ALU.mult, r_, w_)
        self.tt(DEN, T1, T2, ALU.add, r_, w_)
        self.recip(DEN, DEN, r_, w_)
        self.tt(T1, T0, LR, ALU.mult, r_, w_)
        self.tt(T2, LBI, LI, ALU.mult, r_, w_)
        self.tt(T1, T1, T2, ALU.add, r_, w_)
        self.tt(FRE, T1, DEN, ALU.mult, r_, w_)
        self.tt(T1, LBI, LR, ALU.mult, r_, w_)
        self.tt(T2, T0, LI, ALU.mult, r_, w_)
        self.tt(T1, T1, T2, ALU.subtract, r_, w_)
        self.tt(FIM, T1, DEN, ALU.mult, r_, w_)
        self.RHO, self.bS5P = RHO, bS5P
        self.LBR, self.LBI = LBR, LBI

        CS = P.sb("CS", [128, 2, 16, 129])
        bCS = Buf()
        ZB = P.sb("ZB", [128, 2, 16, 128])
        ZC = P.sb("ZC", [128, 2, 16, 128])
        ZT = P.sb("ZT", [128, 2, 16, 128])
        bZ = Buf()
        bZdB = [Buf() for _ in range(4)]
        bZdC = [Buf() for _ in range(4)]
        BCs = P.sb("BCs", [128, 4, 4, 4, 128], BF16)
        bBCs = Buf()
        for l in range(NL):
            thl = TH[:, l * 16:(l + 1) * 16]
            xx = S5F[:].rearrange("p (k t) -> p k t", k=16)
            self.tt(xx, tau[:].unsqueeze(1).to_broadcast([128, 16, 129]), thl.unsqueeze(2).to_broadcast([128, 16, 129]), ALU.mult, [bS5P, bc, bCS], [bCS])
            xflat = S5F[:]
            tmpf = P.sb("s5tmpf%d" % l, [128, 16 * 129]) if l == 0 else tmpf
            sincos(CS[:, 1].rearrange("p k t -> p (k t)"), xflat, 0, [bCS], [bCS], 0.0, tmpf[:], S5I[:])
            sincos(CS[:, 0].rearrange("p k t -> p (k t)"), xflat, 0, [bCS], [bCS], math.pi / 2, tmpf[:], S5I[:])
            P.dma("sp", s_cs[l], CS[:].rearrange("p a k t -> p (a k t)"), reads=[bCS], writes=[b_scs[l]])
            self.memset(ZB[:], 0.0, [bZ] + bZdB)
            self.memset(ZC[:], 0.0, [bZ] + bZdC, eng="dve")
            zi = 0
            for ri, (bsrc, csrc) in enumerate(((bre_d, cre_d), (bim_d, cim_d))):
                for g2 in range(2):
                    for q in range(4):
                        g8 = 2 * q + g2
                        src = bsrc[l].rearrange("(m e) n c -> e n m c", e=8)[g8]
                        P.dma("sp", ZB[g2 * 64:(g2 + 1) * 64, ri, q::4, 16 * g8:16 * g8 + 16], src, writes=[bZdB[zi % 4]], allow_slow_non_contiguous=True)
                        src = csrc[l].rearrange("(m e) c n -> e c m n", e=8)[g8]
                        P.dma("sp", ZC[16 * g8:16 * g8 + 16, ri, q::4, g2 * 64:(g2 + 1) * 64], src, writes=[bZdC[zi % 4]], allow_slow_non_contiguous=True)
                        zi += 1
            fre = FRE[:, l * 16:(l + 1) * 16].unsqueeze(2).to_broadcast([128, 16, 128])
            fim = FIM[:, l * 16:(l + 1) * 16].unsqueeze(2).to_broadcast([128, 16, 128])
            rz = [bZ, bS5P] + bZdB + bZdC
            self.tt(ZT[:, 0], ZB[:, 0], fre, ALU.mult, rz, [bZ])
            self.tt(ZT[:, 1], ZB[:, 1], fim, ALU.mult, rz, [bZ])
            self.tt(ZT[:, 0], ZT[:, 0], ZT[:, 1], ALU.subtract, rz, [bZ])
            self.tt(ZT[:, 1], ZB[:, 1], fre, ALU.mult, rz, [bZ])
            self.tt(ZB[:, 1], ZB[:, 0], fim, ALU.mult, rz, [bZ])
            self.tt(ZT[:, 1], ZT[:, 1], ZB[:, 1], ALU.add, rz, [bZ])
            for jc in range(4):
                for which in range(4):
                    pt, bpt = PS[2 + which % 2], BPS[2 + which % 2]
                    for kk in range(4):
                        k = jc * 4 + kk
                        src = (ZT[:, 0, k, :], ZT[:, 1, k, :], ZC[:, 0, k, :], ZC[:, 1, k, :])[which]
                        self.tr(pt[:, kk * 128:(kk + 1) * 128], src, identf[:], [bZ, bc] + bZdB + bZdC, [bpt])
                    dst = BCs[:, jc, :, which, :]
                    if which == 3:
                        self.act(dst, pt[:].rearrange("p (a b) -> p a b", a=4), AF.Copy, [bpt], [bBCs], scale=-1.0)
                    else:
                        self.act(dst, pt[:].rearrange("p (a b) -> p a b", a=4), AF.Copy, [bpt], [bBCs])
            P.dma("sp", s_bc[l], BCs[:].rearrange("p j a b c -> p j (a b c)"), reads=[bBCs], writes=[b_sbc[l]])

        for l in range(1, NL):
            precast(l)
        print("SBUF bytes/partition at end of setup:", P.sb_bytes)
        if self.cfg.get("stop") == "B":
            P.emit()
            return nc
        P.emit(final=False)
        P.release(setup_mark)
        P.barrier()
        N = TN
        Hb = P.sb("Hb", [128, 4, 64], BF16)
        Pb = Rot(P, "Pb", 3, [128, N + 1])
        Rf = P.sb("Rf", [128, 4, N]); bRf = Buf()
        Kf = P.sb("Kf", [128, 4, N]); bKf = Buf()
        Vf = P.sb("Vf", [128, 4, N]); bVf = Buf()
        VF = P.sb("VF", [128, 4, N]); bVF = Buf()
        WA = P.sb("WA", [128, N]); bWA = Buf()
        TWb = P.sb("TWb", [128, N], BF16); bTW = Buf()
        SIG = P.sb("SIG", [128, 4, N]); bSIG = Buf()
        Aa = P.sb("Aa", [128, 4, N], BF16); bAa = Buf()
        WA2 = P.sb("WA2", [128, RW], BF16); bWA2 = Buf()
        V1s = P.sb("V1s", [128, 4, 32], BF16); bV1 = Buf()
        V2s = P.sb("V2s", [32, RW], BF16); bV2 = Buf()
        Vb = P.sb("Vb", [128, 4, N], BF16); bVb = Buf()
        T32 = P.sb("T32", [32, N], BF16); bT32 = Buf()
        G1 = P.sb("G1", [128, 4, N], BF16); bG1 = Buf()
        G2 = P.sb("G2", [128, 4, N], BF16); bG2 = Buf()
        Ub = P.sb("Ub", [128, 4, N], BF16); bUb = Buf()
        tmp = Rot(P, "tmp", 6, [128, N])
        bOPS = [Buf() for _ in range(4)]
        GL = P.sb("GL", [128, 4, N // 64]); bGL = Buf()
        RRV = P.sb("RRV", [128, 4, N]); bRRV = Buf()
        BRK = P.sb("BRK", [128, 4, 128], BF16); bBRK = Buf()
        YW = SIG; bYW = bSIG
        YSf = P.sb("YSf", [128, 4, N]); bYS2 = Buf()
        WGL = P.sb("WGL", [128, 4, RW], BF16); bWGL = Buf()
        BCt = Rot(P, "BCt", 2, [128, 4, 4, 128], BF16)
        YGb = P.sb("YGb", [128, 4, N], BF16); bYGb = Buf()
        xin = Rot(P, "xin", 2, [128, D])
        full = dict(Rf=Rf, Kf=Kf, Vf=Vf, VF=VF, WA=WA, TWb=TWb, SIG=SIG, Aa=Aa, Vb=Vb, T32=T32, G1=G1, G2=G2, Ub=Ub,
                    RRV=RRV, YGb=YGb)
        prompt_mark = P.mark()
        BI = P.sb("BI", [128, 4, N], BF16)
        KI = P.sb("KI", [128, 4, N], BF16)
        KR = P.sb("KR", [128, 4, 2, N], BF16)
        full.update(BI=BI, KI=KI, KR=KR)
        carry = P.sb("carry", [128, DEPTH, 13])
        bcarry = Buf()
        self.memset(carry[:], 0.0, [bcarry])
        Hst = P.sb("Hst", [128, DEPTH, 4, 64])
        bH = Buf()
        self.memset(Hst[:], 0.0, [bH])
        G0 = P.sb("G0", [128, DEPTH, 2, 16])
        bG0 = Buf()
        self.memset(G0[:], 0.0, [bG0])
        NP = N // 128
        NCH = N // 64
        TM = P.sb("TM", [128, NCH, 3, 4, 64], BF16)
        bTM = [Buf() for _ in range(NCH)]
        ABm = P.sb("ABm", [128, NCH, 4, 128], BF16)
        AKm = P.sb("AKm", [128, NCH, 4, 128], BF16)
        Qa = P.sb("Qa", [128, 1, 2, 2, 4, 64], BF16)
        QTa = P.sb("QTa", [128, 1, 2, 2, 4, 64], BF16)
        Rr = P.sb("Rr", [128, 1, 2, 4, 64])
        Rbb = P.sb("Rbb", [128, NCH, 4, 64], BF16)
        bNEs = [Buf(), Buf()]
        bAB = [Buf() for _ in range(NP)]
        bRbb = [Buf() for _ in range(NP)]
        RHSb = P.sb("RHSb", [128, 4, 64], BF16); bRHS = Buf()
        UUb = P.sb("UUb", [128, 4, 64], BF16); bUU = Buf()
        htmp = P.sb("htmp", [128, 4, 64]); bht = Buf()
        CSl = P.sb("CSl", [128, 2, 16, 129]); bCSl = Buf()
        s5a = Rot(P, "s5a", 8, [128, N])
        s5g = Rot(P, "s5g", 2, [128, 2, N])
        s5h = Rot(P, "s5h", 2, [128, 2, N], BF16)
        gtmp = P.sb("gtmp", [128, 8]); bgt = Buf()
        KTl = P.sb("KTl", [128, 8, MEM], BF16); bKTl = Buf()
        VMl = P.sb("VMl", [128, 2, D], BF16); bVMl = Buf()
        ETb = Rot(P, "ETb", 2, [128, 2, N], BF16)
        rden = Rot(P, "rden", 2, [128, N])
        S5O = P.sb("S5O", [128, 2, 16]); bS5O = Buf()
        s5o2 = P.sb("s5o2", [16, 2, 128]); bs5o2 = Buf()
        wko = P.sb("wko", [64, 4, 128]); bwko = Buf()
        shs = P.sb("shs", [16, 128]); bshs = Buf()
        print("SBUF bytes/partition so far:", P.sb_bytes)

        def load_x(t0):
            for s in range(N // 128):
                xi, bxi = xin.next()
                P.dma("sp", xi[:], xp[t0 + s * 128: t0 + (s + 1) * 128, :], writes=[bxi])
                for half in range(2):
                    pt, bpt = PS[2 + half], BPS[2 + half]
                    for q in range(4):
                        kc = half * 4 + q
                        self.tr(pt[:, q * 128:(q + 1) * 128], xi[:, kc * 128:(kc + 1) * 128], identf[:], [bxi, bc], [bpt])
                    self.cp(xT[:, half * 4:(half + 1) * 4, s * 128:(s + 1) * 128], pt[:].rearrange("p (q m) -> p q m", q=4), [bpt], [bxT], eng="act")

        def store_y(t0):
            self.act(SCRB[:, :, 0:N], xT[:, :, 0:N], AF.Square, [bxT], bSCRk)
            pt, bpt = pdense()
            for kc in range(8):
                self.mm(pt[:, 0:N], onesb[:], SCRB[:, kc, 0:N], kc == 0, kc == 7, [bSCRk[kc], bc], [bpt])
            self.act(rstd[:, 0:N], pt[:, 0:N], AF.Ln, [bpt], [brstd], bias=NORM_EPS, scale=1.0 / D)
            self.act(rstd[:, 0:N], rstd[:, 0:N], AF.Exp, [brstd], [brstd], scale=-0.5)
            for kc in range(8):
                self.stt(xT[:, kc, 0:N], xT[:, kc, 0:N], pc("norm_f", 0, kc), rstd[:, 0:N], ALU.mult, ALU.mult, [bxT, brstd, bprm], [bxT])
            for s in range(N // 128):
                xi, bxi = xin.next()
                for half in range(2):
                    pt, bpt = PS[2 + half], BPS[2 + half]
                    for q in range(4):
                        kc = half * 4 + q
                        self.tr(pt[:, q * 128:(q + 1) * 128], xT[:, kc, s * 128:(s + 1) * 128], identf[:], [bxT, bc], [bpt])
                    self.cp(xi[:, half * 512:(half + 1) * 512], pt[:], [bpt], [bxi], eng="act")
                P.dma("sp", yp[t0 + s * 128: t0 + (s + 1) * 128, :], xi[:], reads=[bxi], final=True)

        def smp_load_shift(l):
            xa, bxa = xin.next()
            xb_, bxb_ = xin.next()
            P.dma("sp", xa[0:16, 0:1024], st_shift[l, :, 0:1024], writes=[bxa])
            P.dma("sp", xb_[0:16, 0:640], st_shift[l, :, 1024:1664], writes=[bxb_])
            pt, bpt = PS[2], BPS[2]
            for j in range(13):
                src = xa[0:16, j * 128:(j + 1) * 128] if j < 8 else xb_[0:16, (j - 8) * 128:(j - 7) * 128]
                self.tr(pt[:, j * 16:(j + 1) * 16], src, identf[0:16, 0:16], [bxa, bxb_, bc], [bpt])
            self.cp(SSH[:, :, :], pt[:, 0:208].rearrange("p (j b) -> p j b", j=13), [bpt], [bSSH], eng="act")

        def smp_store_shift(l):
            xa, bxa = xin.next()
            xb_, bxb_ = xin.next()
            for g in range(4):
                pt, bpt = PS[2 + g % 2], BPS[2 + g % 2]
                js = list(range(g * 4, min(g * 4 + 4, 13)))
                for qi, j in enumerate(js):
                    self.tr(pt[0:16, qi * 128:(qi + 1) * 128], NSH[:, j, :], identf[:], [bNSH, bc], [bpt])
                w_ = len(js) * 128
                if g < 2:
                    self.cp(xa[0:16, g * 512:g * 512 + w_], pt[0:16, 0:w_], [bpt], [bxa], eng="act")
                else:
                    self.cp(xb_[0:16, (g - 2) * 512:(g - 2) * 512 + w_], pt[0:16, 0:w_], [bpt], [bxb_], eng="act")
            P.dma("sp", o_sshift[l, :, 0:1024], xa[0:16, 0:1024], reads=[bxa], final=True)
            P.dma("sp", o_sshift[l, :, 1024:1664], xb_[0:16, 0:640], reads=[bxb_], final=True)

        def smp_bounce_in(c, srcs):
            tms, btms = TMS.next()
            for g3 in range(2):
                pt, bpt = PS[2 + g3], BPS[2 + g3]
                for qq in range(3):
                    src, bsrc = srcs[g3 * 3 + qq]
                    self.tr(pt[0:64, qq * 128:(qq + 1) * 128], src, identf[:], [bsrc, bc], [bpt])
                self.cp(tms[:, g3 * 3:(g3 + 1) * 3, :], pt[0:64, 0:384].rearrange("p (q m) -> p q m", q=3), [bpt], [btms], eng="act")
            P.dma("sp", s_b1[:, :, c * 128:(c + 1) * 128].rearrange("q n m -> n q m"), tms[:, :, :], reads=[btms], writes=[bsb1])

        def smp_wkv(l, YW):
            for q in range(6):
                P.dma("sp", X6[:, q, :, :], s_b1[q].rearrange("(t b) (h j) -> (b h) t j", t=4, h=8), reads=[bsb1], writes=[bX6s[q % 3]])
            P.dma("sp", Ssm[:].rearrange("p i j -> p (i j)"), st_wkv[l].rearrange("b h i j -> (b h) (i j)"), writes=[bSlo, bShi], sembuf=bSlo)

            def bj(a):
                return a.unsqueeze(1).to_broadcast([128, 64, 64])

            def bi_(a):
                return a.unsqueeze(2).to_broadcast([128, 64, 64])
            SPL = 48
            halves = ((slice(0, SPL), "dve", bSlo, bT1lo), (slice(SPL, 64), "pool", bShi, bT1hi))

            def bj(a, n_):
                return a.unsqueeze(1).to_broadcast([128, n_, 64])

            def bi_(a, n_):
                return a.unsqueeze(2).to_broadcast([128, n_, 64])
            for t in range(4):
                r_, w_, k_, v_, kk_, b_ = [X6[:, q, t, :] for q in range(6)]
                for hs_, eng_, bS_, bT_ in halves:
                    n_ = hs_.stop - hs_.start
                    self.tt(T1s[:, hs_, :], Ssm[:, hs_, :], bj(kk_, n_), ALU.mult, [bS_, *bX6s], [bT_], eng=eng_)
                P.op("dve", lambda e: e.tensor_reduce(out=sks[:], in_=T1s[:], axis=mybir.AxisListType.X, op=ALU.add), reads=[bT1lo, bT1hi], writes=[bsk])
                for hs_, eng_, bS_, bT_ in halves:
                    n_ = hs_.stop - hs_.start
                    self.tt(Ssm[:, hs_, :], Ssm[:, hs_, :], bj(w_, n_), ALU.mult, [bS_, *bX6s], [bS_], eng=eng_)
                for hs_, eng_, bS_, bT_ in halves:
                    n_ = hs_.stop - hs_.start
                    self.tt(T1s[:, hs_, :], bi_(sks[:, hs_], n_), bj(b_, n_), ALU.mult, [bsk, *bX6s], [bT_], eng=eng_)
                    self.tt(Ssm[:, hs_, :], Ssm[:, hs_, :], T1s[:, hs_, :], ALU.subtract, [bS_, bT_], [bS_], eng=eng_)
                for hs_, eng_, bS_, bT_ in halves:
                    n_ = hs_.stop - hs_.start
                    self.tt(T1s[:, hs_, :], bi_(v_[:, hs_], n_), bj(k_, n_), ALU.mult, [*bX6s], [bT_], eng=eng_)
                    self.tt(Ssm[:, hs_, :], Ssm[:, hs_, :], T1s[:, hs_, :], ALU.add, [bS_, bT_], [bS_], eng=eng_)
                for hs_, eng_, bS_, bT_ in halves:
                    n_ = hs_.stop - hs_.start
                    self.tt(T1s[:, hs_, :], Ssm[:, hs_, :], bj(r_, n_), ALU.mult, [bS_, *bX6s], [bT_], eng=eng_)
                P.op("dve", lambda e, t=t: e.tensor_reduce(out=Ysm[:, t, :], in_=T1s[:], axis=mybir.AxisListType.X, op=ALU.add), reads=[bT1lo, bT1hi], writes=[bYsm])
            P.dma("sp", o_swkv[l].rearrange("b h i j -> (b h) (i j)"), Ssm[:].rearrange("p i j -> p (i j)"), reads=[bSlo, bShi], sembuf=bSlo, final=True)
            P.dma("sp", s_b2.rearrange("(t b) (h i) -> (b h) t i", t=4, h=8), Ysm[:], reads=[bYsm], writes=[bsb2])
            P.dma("sp", ytm[:], s_b2[:, :], reads=[bsb2], writes=[bytm])
            pt, bpt = PS[2], BPS[2]
            for c in range(4):
                self.tr(pt[:, c * 64:(c + 1) * 64], ytm[:, c * 128:(c + 1) * 128], identf[0:64, 0:64], [bytm, bc], [bpt])
            self.cp(YW[:, :, :], pt[:, 0:256].rearrange("p (c m) -> p c m", c=4), [bpt], [bSIG], eng="act")

        def smp_s5_recur(l, Ub):
            for ri, src in enumerate((st_s5re, st_s5im)):
                for hf in range(2):
                    xa, bxa = xin.next()
                    P.dma("sp", xa[0:16, :], src[l, :, hf * 1024:(hf + 1) * 1024], writes=[bxa])
                    pt, bpt = PS[2 + hf], BPS[2 + hf]
                    for k8 in range(8):
                        self.tr(pt[:, k8 * 16:(k8 + 1) * 16], xa[0:16, k8 * 128:(k8 + 1) * 128], identf[0:16, 0:16], [bxa, bc], [bpt])
                    self.cp(Hs5[:, 0, ri, hf * 8:(hf + 1) * 8, :], pt[:, 0:128].rearrange("p (k b) -> p k b", k=8), [bpt], [bHs5], eng="act")
            for jc in range(4):
                bct, bbct = BCt.next()
                P.dma("sp", bct[:].rearrange("p a b c -> p (a b c)"), s_bc[l, :, jc, :], reads=[b_sbc[l]], writes=[bbct])
                for kk in range(4):
                    k = jc * 4 + kk
                    for ri in range(2):
                        bk = 2 + ri * 2 + k // 8
                        self.mm(PS[bk][:, (k % 8) * 64:(k % 8 + 1) * 64], bct[:, kk, ri, :], Ub[:, jc, :], True, True, [bbct, bUb], [BPS[bk]])
            lbr = LBR[:, l * 16:(l + 1) * 16].unsqueeze(2).to_broadcast([128, 16, 16])
            lbi = LBI[:, l * 16:(l + 1) * 16].unsqueeze(2).to_broadcast([128, 16, 16])
            rr_ = [bHs5, bS5P, bs5t]
            for t in range(4):
                cur, nxt = t % 2, (t + 1) % 2
                hre, him = Hs5[:, cur, 0], Hs5[:, cur, 1]
                nre, nim = Hs5[:, nxt, 0], Hs5[:, nxt, 1]
                self.tt(s5t[:, 0], him, lbi, ALU.mult, rr_, [bs5t])
                self.tt(s5t[:, 1], hre, lbi, ALU.mult, rr_, [bs5t])
                self.tt(nre, hre, lbr, ALU.mult, rr_, [bHs5])
                self.tt(nim, him, lbr, ALU.mult, rr_, [bHs5])
                self.tt(nre, nre, s5t[:, 0], ALU.subtract, rr_, [bHs5])
                self.tt(nim, nim, s5t[:, 1], ALU.add, rr_, [bHs5])
                for ri, dst in ((0, nre), (1, nim)):
                    for hf in range(2):
                        bk = 2 + ri * 2 + hf
                        bu = PS[bk][:, :].rearrange("p (k n) -> p k n", k=8)[:, :, t * 16:(t + 1) * 16]
                        self.tt(dst[:, hf * 8:(hf + 1) * 8, :], dst[:, hf * 8:(hf + 1) * 8, :], bu, ALU.add, [bHs5, BPS[bk]], [bHs5])
                self.cp(HsT[:, 0, :, t * 16:(t + 1) * 16], nre, [bHs5], [bHsT], eng="pool")
                self.cp(HsT[:, 1, :, t * 16:(t + 1) * 16], nim, [bHs5], [bHsT], eng="pool")
            for ri, dstd in ((0, o_ss5re), (1, o_ss5im)):
                for g in range(4):
                    pt, bpt = PS[6], BPS[6]
                    for kk in range(4):
                        self.tr(pt[0:16, kk * 128:(kk + 1) * 128], Hs5[:, 0, ri, g * 4 + kk, :], identf[:], [bHs5, bc], [bpt])
                    xa, bxa = xin.next()
                    self.cp(xa[0:16, 0:512], pt[0:16, :], [bpt], [bxa], eng="act")
                    P.dma("sp", dstd[l, :, g * 512:(g + 1) * 512], xa[0:16, 0:512], reads=[bxa], final=True)

        def smp_attn(l):
            pdn, bpdn = PS[5], BPS[5]
            po, bpo = PS[6], BPS[6]
            t1flat = T1s[:].rearrange("p i j -> p (i j)")
            Kalt = t1flat[:, 0:2048].rearrange("p (a d) -> p a d", a=2)
            Valt = t1flat[:, 3072:4096].bitcast(BF16).rearrange("p (a d) -> p a d", a=2)
            for b in range(NB_S):
                if b % 2 == 0:
                    Kb_, bKb_, Vb_, bVb_ = Kf32, [bKf32], Vs, [bVs]
                else:
                    Kb_, bKb_, Vb_, bVb_ = Kalt, [bT1lo], Valt, [bT1hi]
                P.dma("sp", Kb_[:, :, :], ck[l, b].rearrange("(mc p) d -> p mc d", p=128), writes=bKb_)
                P.dma("pool", Vb_[:, :, :], cv[l, b].rearrange("(mc p) d -> p mc d", p=128), writes=bVb_)
                for mc in range(2):
                    for g4 in range(2):
                        pt, bpt = PS[2 + g4], BPS[2 + g4]
                        for q in range(4):
                            dc = g4 * 4 + q
                            self.tr(pt[:, q * 128:(q + 1) * 128], Kb_[:, mc, dc * 128:(dc + 1) * 128], identf[:], bKb_ + [bc], [bpt])
                        self.cp(KTs[:, g4 * 4:(g4 + 1) * 4, mc * 128:(mc + 1) * 128], pt[:].rearrange("p (q m) -> p q m", q=4), [bpt], [bKTs], eng="act")
                psc, bpsc = (PS[4], BPS[4]) if b % 2 == 0 else (PS[7], BPS[7])
                for mc in range(2):
                    for h in range(4):
                        c0_ = (mc * 4 + h) * 4
                        for dcl in range(2):
                            dc = 2 * h + dcl
                            self.mm(psc[:, c0_:c0_ + 4], KTs[:, dc, mc * 128:(mc + 1) * 128], SCRB[:, dc, b:64:16], dcl == 0, dcl == 1, [bKTs, bSCRk[dc]], [bpsc])
                self.act(ETs[:, b, :], psc[:, 0:32], AF.Exp, [bpsc], [bETs], scale=1.0 / 16.0)
                for mc in range(2):
                    self.mm(pdn[:, b * 16:(b + 1) * 16], onesb[:], ETs[:, b, mc * 16:(mc + 1) * 16], mc == 0, mc == 1, [bETs, bc], [bpdn])
                for dc in range(8):
                    h = dc // 2
                    for mc in range(2):
                        c0_ = (mc * 4 + h) * 4
                        self.mm(po[:, dc * 64 + b:dc * 64 + 64:16], Vb_[:, mc, dc * 128:(dc + 1) * 128], ETs[:, b, c0_:c0_ + 4], mc == 0, mc == 1, bVb_ + [bETs], [bpo])
            self.act(rds[:, :, :], pdn[:, 0:256].rearrange("p (b x) -> p b x", b=16), AF.Ln, [bpdn], [brds])
            self.act(rds[:, :, :], rds[:, :, :], AF.Exp, [brds], [brds], scale=-1.0)
            for dc in range(8):
                h = dc // 2
                self.tt(ACTB[:, dc, 0:64].rearrange("p (t b) -> p t b", t=4), po[:, dc * 64:(dc + 1) * 64].rearrange("p (t b) -> p t b", t=4),
                        rds[:, :, h * 4:(h + 1) * 4].rearrange("p b t -> p t b"), ALU.mult, [bpo, brds], [bACTk[dc]])

        def layer(l, last_tile, N, smp):
            Rf, Kf, Vf, VF, SIG, Aa, Vb, G1, G2, Ub, RRV, YGb = [full[n_][:, :, 0:N] for n_ in ("Rf", "Kf", "Vf", "VF", "SIG", "Aa", "Vb", "G1", "G2", "Ub", "RRV", "YGb")]
            WA, TWb, T32 = full["WA"][:, 0:N], full["TWb"][:, 0:N], full["T32"][:, 0:N]
            YW = SIG
            YS = YSf[:, :, 0:N]
            if not smp:
                BI, KI = full["BI"][:, :, 0:N], full["KI"][:, :, 0:N]
                KR = full["KR"][:, :, :, 0:N]

            def tnext():
                t_, b_ = tmp.next()
                return t_[:, 0:N], b_
            rmsnorm("norm_mix", l, N)
            if smp:
                smp_load_shift(l)
            P.dma("pool", WA2[0:64, :], w2[l], writes=[bWA2])
            P.dma("pool", WA2[64:128, :], a2[l], writes=[bWA2])
            if l > 0:
                P.dma("pool", V1s[:], v1[l - 1].rearrange("(c p) r -> p c r", p=128), writes=[bV1])
                P.dma("pool", V2s[:], v2[l - 1], writes=[bV2])
            P.dma("pool", WGL[:], wglu[l].rearrange("(c p) r -> p c r", p=128), writes=[bWGL])
            if not smp:
                P.dma("sp", CSl[:].rearrange("p a k t -> p (a k t)"), s_cs[l], reads=[b_scs[l]], writes=[bCSl])
            vdst, bvdst = (VF, bVF) if l == 0 else (Vf, bVf)
            def g_proj(order):
                for grp in order:
                    wt, bw = wload_t(s_win[l, grp], 640, b_swin[l])
                    for jj in range(5):
                        j = grp * 5 + jj
                        pt, bpt = pdense()
                        for kc in range(8):
                            self.mm(pt[:, 0:N], wt[:, kc, jj * 128:(jj + 1) * 128], ACTB[:, kc, 0:N], kc == 0, kc == 7, [bw, bACTk[kc]], [bpt])
                        if j < 13:
                            pb, bpb = Pb.next()
                            if not smp:
                                self.cp(pb[:, 0:1], carry[:, l, j:j + 1], [bcarry], [bpb], eng="pool")
                                self.cp(pb[:, 1:N + 1], pt[:, 0:N], [bpt], [bpb], eng="act")
                                self.cp(carry[:, l, j:j + 1], pb[:, N:N + 1], [bpb], [bcarry], eng="pool")
                                prev_, cur_ = pb[:, 0:N], pb[:, 1:N + 1]
                            else:
                                self.cp(pb[:, 0:16], SSH[:, j, :], [bSSH], [bpb], eng="pool")
                                self.cp(pb[:, 16:80], pt[:, 0:64], [bpt], [bpb], eng="act")
                                self.cp(NSH[:, j, :], pb[:, 64:80], [bpb], [bNSH], eng="pool")
                                prev_, cur_ = pb[:, 0:64], pb[:, 16:80]
                            tm_, btm_ = tnext()
                            self.act(tm_[:, 0:N], prev_, AF.Identity, [bpb, bprm], [btm_], scale=pc("mu_shift", l, j))
                            if j < 4:
                                dst, bd = Rf[:, j, :], bRf
                            elif j < 8:
                                dst, bd = Kf[:, j - 4, :], bKf
                            elif j < 12:
                                dst, bd = vdst[:, j - 8, :], bvdst
                            else:
                                dst, bd = WA[:, :], bWA
                            self.stt(dst, cur_, OMM[:, l * 13 + j:l * 13 + j + 1], tm_[:, 0:N], ALU.mult, ALU.add, [bpb, btm_, bprm], [bd])
                        elif j < 17:
                            self.act(G1[:, j - 13, :], pt[:, 0:N], AF.Silu, [bpt], [bG1])
                        elif j < 21:
                            self.act(Ub[:, j - 17, :], pt[:, 0:N], AF.Copy, [bpt], [bUb])
                        else:
                            self.act(G2[:, j - 21, :], pt[:, 0:N], AF.Silu, [bpt], [bG2])
                        yield
            def g_rwkv():
                self.act(TWb[0:64, :], WA[0:64, :], AF.Tanh, [bWA], [bTW])
                self.cp(TWb[64:128, :], WA[64:128, :], [bWA], [bTW], eng="pool")
                for c in range(4):
                    pt, bpt = pdense()
                    self.mm(pt[:, 0:N], WA2[0:64, c * 128:(c + 1) * 128], TWb[0:64, :], True, True, [bWA2, bTW], [bpt])
                    self.act(SIG[:, c, :], pt[:, 0:N], AF.Sigmoid, [bpt, bprm], [bSIG], bias=pc("w0", l, c))
                    pt, bpt = pdense()
                    self.mm(pt[:, 0:N], WA2[64:128, c * 128:(c + 1) * 128], TWb[64:128, :], True, True, [bWA2, bTW], [bpt])
                    self.act(Aa[:, c, :], pt[:, 0:N], AF.Sigmoid, [bpt, bprm], [bAa], bias=pc("a0", l, c))
                yield
                if l > 0:
                    self.cp(Vb[:, :, :], Vf[:, :, :], [bVf], [bVb], eng="pool")
                    pt, bpt = pdense()
                    for c in range(4):
                        self.mm(pt[0:32, 0:N], V1s[:, c, :], Vb[:, c, :], c == 0, c == 3, [bV1, bVb], [bpt])
                    self.cp(T32[:, :], pt[0:32, 0:N], [bpt], [bT32], eng="act")
                    for c in range(4):
                        pt, bpt = pdense()
                        self.mm(pt[:, 0:N], V2s[0:32, c * 128:(c + 1) * 128], T32[0:32, :], True, True, [bV2, bT32], [bpt])
                        g_, bg_ = tnext()
                        self.act(g_[:, :], pt[:, 0:N], AF.Sigmoid, [bpt, bprm], [bg_], bias=pc("v0", l - 1, c))
                        d_, bd_ = tnext()
                        self.tt(d_[:, :], VF[:, c, :], Vf[:, c, :], ALU.subtract, [bVF, bVf], [bd_])
                        self.tt(d_[:, :], d_[:, :], g_[:, :], ALU.mult, [bd_, bg_], [bd_])
                        self.tt(Vf[:, c, :], Vf[:, c, :], d_[:, :], ALU.add, [bVf, bd_], [bVf])
                self.cp(Vb[:, :, :], vdst[:, :, :], [bvdst], [bVb], eng="pool")
                yield
                for c in range(4):
                    self.ts(BRK[:, c, :], bonesb[:], pc("r_k", l, c), ALU.mult, [bc, bprm], [bBRK], eng="pool")
                for c in range(4):
                    bo = bOPS[c]
                    kkr, bkkr = tnext()
                    self.act(kkr[:, :], Kf[:, c, :], AF.Identity, [bKf, bprm], [bkkr], scale=pc("k_k", l, c))
                    sq, bsq = tnext()
                    self.act(sq[:, :], kkr[:, :], AF.Square, [bkkr], [bsq])
                    pt, bpt = pdense()
                    self.mm(pt[:, 0:N], bonesf[:], sq[:, :], True, True, [bsq, bc], [bpt])
                    self.act(sq[:, :], pt[:, 0:N], AF.Ln, [bpt], [bsq], scale=64.0, bias=1e-12)
                    self.act(sq[:, :], sq[:, :], AF.Exp, [bsq], [bsq], scale=-0.5)
                    self.tt(kkr[:, :], kkr[:, :], sq[:, :], ALU.mult, [bkkr, bsq], [bkkr])
                    bq, bbq = tnext()
                    self.tt(bq[:, :], kkr[:, :], Aa[:, c, :], ALU.mult, [bkkr, bAa], [bbq])
                    kp, bkp = tnext()
                    self.act(kp[:, :], Aa[:, c, :], AF.Identity, [bAa, bprm], [bkp], scale=pc("k_a", l, c), bias=OMK[:, l * 4 + c:l * 4 + c + 1])
                    self.tt(kp[:, :], kp[:, :], Kf[:, c, :], ALU.mult, [bkp, bKf], [bkp])
                    rk, brk = tnext()
                    self.tt(sq[:, :], Rf[:, c, :], kp[:, :], ALU.mult, [bRf, bkp], [bsq])
                    self.cp(TWb[:, :], sq[:, :], [bsq], [bTW], eng="pool")
                    pt, bpt = pdense()
                    self.mm(pt[:, 0:N], BRK[:, c, :], TWb[:, :], True, True, [bBRK, bTW], [bpt])
                    self.tt(RRV[:, c, :], pt[:, 0:N], vdst[:, c, :], ALU.mult, [bpt, bvdst], [bRRV])
                    if smp:
                        gam, bgam = rk, brk
                        self.act(gam[:, :], SIG[:, c, :], AF.Exp, [bSIG], [bgam], scale=-C0)
                        smp_bounce_in(c, [(Rf[:, c, :], bRf), (gam[:, :], bgam), (kp[:, :], bkp), (vdst[:, c, :], bvdst), (kkr[:, :], bkkr), (bq[:, :], bbq)])
                        yield
                        continue
                    cum, bcum = rk, brk
                    self.scan(cum[:, :], cmask[:, 0:N], SIG[:, c, :], 0.0, [bc, bSIG], [bcum])
                    gam, bgam = sq, bsq
                    self.act(gam[:, :], cum[:, :], AF.Exp, [bcum], [bgam], scale=-C0)
                    self.cp(GL[:, c, :], gam[:, :].rearrange("p (a b) -> p a b", b=64)[:, :, 63], [bgam], [bGL], eng="pool")
                    self.tt(KR[:, c, 1, :], Rf[:, c, :], gam[:, :], ALU.mult, [bRf, bgam], [bo])
                    self.tt(gam[:, :], cum[:, :], SIG[:, c, :], ALU.subtract, [bcum, bSIG], [bgam])
                    self.act(gam[:, :], gam[:, :], AF.Exp, [bgam], [bgam], scale=-C0)
                    self.tt(KR[:, c, 0, :], kkr[:, :], gam[:, :], ALU.mult, [bkkr, bgam], [bo])
                    self.act(gam[:, :], cum[:, :], AF.Exp, [bcum], [bgam], scale=C0)
                    self.tt(BI[:, c, :], bq[:, :], gam[:, :], ALU.mult, [bbq, bgam], [bo])
                    self.tt(KI[:, c, :], kp[:, :], gam[:, :], ALU.mult, [bkp, bgam], [bo])
                    yield
                if smp:
                    smp_wkv(l, YW)
                for ch in range(0 if smp else NCH):
                    cs = slice(ch * 64, (ch + 1) * 64)
                    pt, bpt = pdense()
                    ptb = pt[:].bitcast(BF16)
                    for h2 in (0, 1):
                        hb = h2 * 64
                        P.pe_fence()
                        for xi, (src, bsrc) in enumerate(((Vb, [bVb]), (BI, bOPS), (KI, bOPS))):
                            for c in range(4):
                                cl = (xi * 4 + c) * 64
                                self.tr(ptb[hb:hb + 64, cl:cl + 64], src[hb:hb + 64, c, cs], identb[hb:hb + 64, hb:hb + 64], list(bsrc) + [bc], [bpt])
                    self.cp(TM[:, ch, :, :, :], ptb[:, 0:768].rearrange("p (x c m) -> p x c m", x=3, c=4), [bpt], [bTM[ch]], eng="act")
                    yield
                for pp in range(0 if smp else NP):
                    st = 0
                    bne = bNEs[st]
                    for par in range(2):
                        ch = pp * 2 + par
                        cs = slice(ch * 64, (ch + 1) * 64)
                        pab, bpab = PS[2], BPS[2]
                        pak, bpak = PS[3], BPS[3]
                        pq, bpq = PS[4], BPS[4]
                        for h2 in (0, 1):
                            hb = h2 * 64
                            P.pe_fence()
                            for c in range(4):
                                self.mm(pab[hb:hb + 64, c * 128:(c + 1) * 128], BI[hb:hb + 64, c, cs], KR[hb:hb + 64, c, :, cs], True, True, [bOPS[c]], [bpab])
                                self.mm(pak[hb:hb + 64, c * 128:(c + 1) * 128], KI[hb:hb + 64, c, cs], KR[hb:hb + 64, c, :, cs], True, True, [bOPS[c]], [bpak])
                                self.mm(pq[hb:hb + 64, c * 64:(c + 1) * 64], KR[hb:hb + 64, c, 0, cs], BI[hb:hb + 64, c, cs], True, True, [bOPS[c]], [bpq])
                        mb = mska[:, :].unsqueeze(1).to_broadcast([128, 4, 128])
                        self.tt(ABm[:, ch, :, :], pab[:, :].rearrange("p (c m) -> p c m", c=4), mb, ALU.mult, [bpab, bc], [bAB[pp]])
                        self.tt(AKm[:, ch, :, :], pak[:, :].rearrange("p (c m) -> p c m", c=4), mb, ALU.mult, [bpak, bc], [bAB[pp]])
                        ml = mskl[:, :].unsqueeze(1).to_broadcast([128, 4, 64])
                        self.stt(QTa[:, st, 0, par, :, :], pq[:, 0:256].rearrange("p (c m) -> p c m", c=4), -1.0, ml, ALU.mult, ALU.mult, [bpq, bc], [bne])
                        self.ts(Qa[:, st, 0, par, :, :], ABm[:, ch, :, 0:64], -1.0, ALU.mult, [bAB[pp]], [bne])
                        for tb in (0, 64):
                            idb = identf[tb:tb + 64, tb:tb + 64].unsqueeze(1).to_broadcast([64, 4, 64])
                            self.tt(Rr[tb:tb + 64, st, par, :, :], Qa[tb:tb + 64, st, 0, par, :, :], idb, ALU.add, [bne, bc], [bne])
                        yield
                    chs = slice(pp * 2, pp * 2 + 2)
                    self.cp(Rbb[:, chs, :, :], Rr[:, st, :, :, :], [bne], [bRbb[pp]], eng="pool")

                    def v4(ap):
                        return ap.rearrange("p (a c m) -> p a c m", a=2, c=4)
                    for lev in range(1, 6):
                        a_, b_ = (lev - 1) % 2, lev % 2
                        pq1, bpq1 = PS[2], BPS[2]
                        pq2, bpq2 = PS[3], BPS[3]
                        pq3, bpq3 = PS[4], BPS[4]
                        for h2 in (0, 1):
                            hb = h2 * 64
                            P.pe_fence()
                            for par in range(2):
                                for c in range(4):
                                    hs = slice((par * 4 + c) * 64, (par * 4 + c + 1) * 64)
                                    if lev < 5:
                                        self.mm(pq1[hb:hb + 64, hs], QTa[hb:hb + 64, st, a_, par, c, :], Qa[hb:hb + 64, st, a_, par, c, :], True, True, [bne], [bpq1])
                                    self.mm(pq2[hb:hb + 64, hs], Qa[hb:hb + 64, st, a_, par, c, :], QTa[hb:hb + 64, st, a_, par, c, :], True, True, [bne], [bpq2])
                        if lev < 5:
                            self.cp(Qa[:, st, b_, :, :, :], v4(pq1[:]), [bpq1], [bne], eng="act")
                        self.cp(QTa[:, st, b_, :, :, :], v4(pq2[:]), [bpq2], [bne], eng="act")
                        yield
                        for h2 in (0, 1):
                            hb = h2 * 64
                            P.pe_fence()
                            for par in range(2):
                                for c in range(4):
                                    hs = slice((par * 4 + c) * 64, (par * 4 + c + 1) * 64)
                                    self.mm(pq3[hb:hb + 64, hs], QTa[hb:hb + 64, st, b_, par, c, :], Rbb[hb:hb + 64, pp * 2 + par, c, :], True, True, [bne, bRbb[pp]], [bpq3])
                        self.tt(Rr[:, st, :, :, :], Rr[:, st, :, :, :], v4(pq3[:]), ALU.add, [bne, bpq3], [bne])
                        self.cp(Rbb[:, chs, :, :], Rr[:, st, :, :, :], [bne], [bRbb[pp]], eng="pool")
                        yield
                if not smp:
                    self.cp(Hb[:], Hst[:, l, :, :], [bH], [bH], eng="pool")
                for ch in range(0 if smp else NCH):
                    pp = ch // 2
                    cs = slice(ch * 64, (ch + 1) * 64)
                    pr, bpr = PS[2], BPS[2]
                    for h2 in (0, 1):
                        hb = h2 * 64
                        P.pe_fence()
                        for c in range(4):
                            hs = slice(c * 64, (c + 1) * 64)
                            self.mm(pr[hb:hb + 64, hs], KR[hb:hb + 64, c, 0, cs], Hb[hb:hb + 64, c, :], True, False, [bOPS[c], bH], [bpr])
                            self.mm(pr[hb:hb + 64, hs], AKm[hb:hb + 64, ch, c, 0:64], TM[hb:hb + 64, ch, 0, c, :], False, True, [bAB[pp], bTM[ch]], [bpr])
                    self.act(RHSb[:, :, :], pr[:, 0:256].rearrange("p (c m) -> p c m", c=4), AF.Copy, [bpr], [bRHS], scale=-1.0)
                    yield
                    pu, bpu = PS[3], BPS[3]
                    for h2 in (0, 1):
                        hb = h2 * 64
                        P.pe_fence()
                        for c in range(4):
                            hs = slice(c * 64, (c + 1) * 64)
                            self.mm(pu[hb:hb + 64, hs], Rbb[hb:hb + 64, ch, c, :], RHSb[hb:hb + 64, c, :], True, True, [bRbb[pp], bRHS], [bpu])
                    self.cp(UUb[:, :, :], pu[:, 0:256].rearrange("p (c m) -> p c m", c=4), [bpu], [bUU], eng="act")
                    yield
                    py, bpy = PS[4], BPS[4]
                    ph, bph = PS[4][:, 256:512], BPS[4]
                    for h2 in (0, 1):
                        hb = h2 * 64
                        P.pe_fence()
                        for c in range(4):
                            hs = slice(c * 64, (c + 1) * 64)
                            self.mm(py[hb:hb + 64, hs], Hb[hb:hb + 64, c, :], KR[hb:hb + 64, c, 1, cs], True, False, [bH, bOPS[c]], [bpy])
                            self.mm(py[hb:hb + 64, hs], UUb[hb:hb + 64, c, :], ABm[hb:hb + 64, ch, c, 64:128], False, False, [bUU, bAB[pp]], [bpy])
                            self.mm(py[hb:hb + 64, hs], TM[hb:hb + 64, ch, 0, c, :], AKm[hb:hb + 64, ch, c, 64:128], False, True, [bTM[ch], bAB[pp]], [bpy])
                            self.mm(ph[hb:hb + 64, hs], TM[hb:hb + 64, ch, 1, c, :], UUb[hb:hb + 64, c, :], True, False, [bTM[ch], bUU], [bph])
                            self.mm(ph[hb:hb + 64, hs], TM[hb:hb + 64, ch, 2, c, :], TM[hb:hb + 64, ch, 0, c, :], False, True, [bTM[ch]], [bph])
                    self.cp(YW[:, :, cs], py[:, 0:256].rearrange("p (c m) -> p c m", c=4), [bpy], [bYW], eng="act")
                    self.tt(htmp[:], ph[:, 0:256].rearrange("p (c m) -> p c m", c=4), Hst[:, l, :, :], ALU.add, [bph, bH], [bht])
                    self.tt(Hst[:, l, :, :], htmp[:], GL[:, :, ch:ch + 1].to_broadcast([128, 4, 64]), ALU.mult, [bht, bGL], [bH])
                    self.cp(Hb[:], Hst[:, l, :, :], [bH], [bH], eng="act")
                    yield
                for c in range(4):
                    pt, bpt = pdense()
                    self.mm(pt[:, 0:N], bonesf[:], YW[:, c, :], True, True, [bYW, bc], [bpt])
                    yc, byc = tnext()
                    self.tt(yc[:, :], YW[:, c, :], pt[:, 0:N], ALU.subtract, [bYW, bpt], [byc])
                    sq, bsq = tnext()
                    self.act(sq[:, :], yc[:, :], AF.Square, [byc], [bsq])
                    pt, bpt = pdense()
                    self.mm(pt[:, 0:N], bonesf[:], sq[:, :], True, True, [bsq, bc], [bpt])
                    self.act(sq[:, :], pt[:, 0:N], AF.Ln, [bpt], [bsq], bias=GN_EPS)
                    self.act(sq[:, :], sq[:, :], AF.Exp, [bsq], [bsq], scale=-0.5)
                    self.tt(yc[:, :], yc[:, :], sq[:, :], ALU.mult, [byc, bsq], [byc])
                    self.act(yc[:, :], yc[:, :], AF.Identity, [byc, bprm], [byc], scale=pc("gn_w", l, c), bias=pc("gn_b", l, c))
                    self.tt(yc[:, :], yc[:, :], RRV[:, c, :], ALU.add, [byc, bRRV], [byc])
                    self.tt(ACTB[:, c, 0:N], yc[:, :], G1[:, c, :], ALU.mult, [byc, bG1], [bACTk[c]])
                    yield
            def g_s5():
                if smp:
                    smp_s5_recur(l, Ub)
                pyc, bpyc = PS[7], BPS[7]

                def post(jc):
                    self.stt(YS[:, jc, :], Ub[:, jc, :], pc("d_skip", l, jc), pyc[:, 0:N], ALU.mult, ALU.add, [bUb, bpyc, bprm], [bYS2])
                    self.act(YS[:, jc, :], YS[:, jc, :], AF.Gelu, [bYS2], [bYS2])
                    self.cp(YGb[:, jc, :], YS[:, jc, :], [bYS2], [bYGb], eng="pool")
                if smp:
                    for jc in range(4):
                        bct, bbct = BCt.next()
                        P.dma("sp", bct[:].rearrange("p a b c -> p (a b c)"), s_bc[l, :, jc, :], reads=[b_sbc[l]], writes=[bbct])
                        for kk in range(4):
                            k = jc * 4 + kk
                            self.mm(pyc[:, 0:N], bct[:, kk, 2, :], HsT[:, 0, k, :], kk == 0, False, [bbct, bHsT], [bpyc])
                            self.mm(pyc[:, 0:N], bct[:, kk, 3, :], HsT[:, 1, k, :], False, kk == 3, [bbct, bHsT], [bpyc])
                        post(jc)
                        yield
                else:
                    NQ = N // 128

                    def v3(ap):
                        return ap.rearrange("p (q t) -> p q t", t=128)
                    S_ = {}
                    bcts = {}

                    def tabs(k):
                        return (CSl[:, 0, k, 0:128].unsqueeze(1).to_broadcast([128, NQ, 128]),
                                CSl[:, 1, k, 0:128].unsqueeze(1).to_broadcast([128, NQ, 128]))

                    def a_mm(k):
                        jc, kk = divmod(k, 4)
                        if kk == 0:
                            bct, bbct = BCt.next()
                            P.dma("sp", bct[:].rearrange("p a b c -> p (a b c)"), s_bc[l, :, jc, :], reads=[b_sbc[l]], writes=[bbct])
                            bcts[jc] = (bct, bbct)
                        bct, bbct = bcts[jc]
                        bank = 5 + k % 2
                        pbr, pbi, bpb = PS[bank][:, 0:256], PS[bank][:, 256:512], BPS[bank]
                        self.mm(pbr[:, 0:N], bct[:, kk, 0, :], Ub[:, jc, :], True, True, [bbct, bUb], [bpb])
                        self.mm(pbi[:, 0:N], bct[:, kk, 1, :], Ub[:, jc, :], True, True, [bbct, bUb], [bpb])
                        S_[k] = dict(pbr=pbr, pbi=pbi, bpb=bpb, t=[s5a.next() for _ in range(4)], g=s5g.next(), h=s5h.next())

                    def a_dve(k):
                        d = S_[k]
                        cosb, sinb = tabs(k)
                        (t1, bt1), (t2, bt2), (t3, bt3), (t4, bt4) = d["t"]
                        self.tt(v3(t1[:, :]), v3(d["pbr"][:, 0:N]), cosb, ALU.mult, [d["bpb"], bCSl], [bt1])
                        self.tt(v3(t2[:, :]), v3(d["pbi"][:, 0:N]), sinb, ALU.mult, [d["bpb"], bCSl], [bt2])
                        self.tt(v3(t3[:, :]), v3(d["pbi"][:, 0:N]), cosb, ALU.mult, [d["bpb"], bCSl], [bt3])
                        self.tt(v3(t4[:, :]), v3(d["pbr"][:, 0:N]), sinb, ALU.mult, [d["bpb"], bCSl], [bt4])

                    def a_pool(k):
                        (t1, bt1), (t2, bt2), (t3, bt3), (t4, bt4) = S_[k]["t"]
                        self.tt(t1[:, :], t1[:, :], t2[:, :], ALU.add, [bt1, bt2], [bt1], eng="pool")
                        self.tt(t3[:, :], t3[:, :], t4[:, :], ALU.subtract, [bt3, bt4], [bt3], eng="pool")

                    def a_scan(k):
                        d = S_[k]
                        (t1, bt1), (t2, bt2), (t3, bt3), (t4, bt4) = d["t"]
                        g, bg = d["g"]
                        rho = RHO[:, l * 16 + k:l * 16 + k + 1]
                        cr = CSl[:, 0, k, 128:129]
                        ci = CSl[:, 1, k, 128:129]
                        for q in range(NQ):
                            qs = slice(q * 128, (q + 1) * 128)
                            self.scan(g[:, 0, qs], rho.to_broadcast([128, 128]), t1[:, qs], G0[:, l, 0, k:k + 1], [bt1, bS5P, bG0], [bg])
                            self.scan(g[:, 1, qs], rho.to_broadcast([128, 128]), t3[:, qs], G0[:, l, 1, k:k + 1], [bt3, bS5P, bG0], [bg])
                            e1 = q * 128 + 127
                            self.ts(gtmp[:, 0:1], g[:, 1, e1:e1 + 1], ci, ALU.mult, [bg, bCSl], [bgt])
                            self.ts(gtmp[:, 1:2], g[:, 1, e1:e1 + 1], cr, ALU.mult, [bg, bCSl], [bgt])
                            self.stt(G0[:, l, 0, k:k + 1], g[:, 0, e1:e1 + 1], cr, gtmp[:, 0:1], ALU.mult, ALU.subtract, [bg, bgt, bCSl], [bG0])
                            self.stt(G0[:, l, 1, k:k + 1], g[:, 0, e1:e1 + 1], ci, gtmp[:, 1:2], ALU.mult, ALU.add, [bg, bgt, bCSl], [bG0])

                    def b_pool(k):
                        d = S_[k]
                        cosb, sinb = tabs(k)
                        (t1, bt1), (t2, bt2), (t3, bt3), (t4, bt4) = d["t"]
                        g, bg = d["g"]
                        self.tt(v3(t1[:, :]), v3(g[:, 0, :]), cosb, ALU.mult, [bg, bCSl], [bt1], eng="pool")
                        self.tt(v3(t2[:, :]), v3(g[:, 1, :]), sinb, ALU.mult, [bg, bCSl], [bt2], eng="pool")
                        self.tt(v3(t3[:, :]), v3(g[:, 0, :]), sinb, ALU.mult, [bg, bCSl], [bt3], eng="pool")
                        self.tt(v3(t4[:, :]), v3(g[:, 1, :]), cosb, ALU.mult, [bg, bCSl], [bt4], eng="pool")

                    def b_dve(k):
                        d = S_[k]
                        jc, kk = divmod(k, 4)
                        bct, bbct = bcts[jc]
                        (t1, bt1), (t2, bt2), (t3, bt3), (t4, bt4) = d["t"]
                        hh_, bhh = d["h"]
                        self.tt(hh_[:, 0, :], t1[:, :], t2[:, :], ALU.subtract, [bt1, bt2], [bhh])
                        self.tt(hh_[:, 1, :], t3[:, :], t4[:, :], ALU.add, [bt3, bt4], [bhh])
                        if last_tile:
                            self.tt(S5O[:, 0, k:k + 1], t1[:, N - 1:N], t2[:, N - 1:N], ALU.subtract, [bt1, bt2], [bS5O])
                            self.tt(S5O[:, 1, k:k + 1], t3[:, N - 1:N], t4[:, N - 1:N], ALU.add, [bt3, bt4], [bS5O])
                        self.mm(pyc[:, 0:N], bct[:, kk, 2, :], hh_[:, 0, :], kk == 0, False, [bbct, bhh], [bpyc])
                        self.mm(pyc[:, 0:N], bct[:, kk, 3, :], hh_[:, 1, :], False, kk == 3, [bbct, bhh], [bpyc])
                        if kk == 3:
                            post(jc)
                        del S_[k]
                    a_mm(0)
                    a_dve(0)
                    a_pool(0)
                    a_scan(0)
                    yield
                    for k in range(16):
                        b_pool(k)
                        if k + 1 < 16:
                            a_mm(k + 1)
                            a_dve(k + 1)
                            a_pool(k + 1)
                        yield
                        b_dve(k)
                        if k + 1 < 16:
                            a_scan(k + 1)
                        yield
                for jc in range(4):
                    pt, bpt = pdense()
                    for kc in range(4):
                        self.mm(pt[:, 0:N], WGL[:, kc, jc * 128:(jc + 1) * 128], YGb[:, kc, :], kc == 0, kc == 3, [bWGL, bYGb], [bpt])
                    sg, bsg = tnext()
                    self.act(sg[:, :], pt[:, 0:N], AF.Sigmoid, [bpt], [bsg])
                    self.tt(sg[:, :], sg[:, :], YS[:, jc, :], ALU.mult, [bsg, bYS2], [bsg])
                    self.tt(ACTB[:, 4 + jc, 0:N], sg[:, :], G2[:, jc, :], ALU.mult, [bsg, bG2], [bACTk[4 + jc]])
                    yield
            def rr(gs_, until=None):
                alive = list(gs_)
                while alive and (until is None or until in alive):
                    for g_ in list(alive):
                        try:
                            next(g_)
                        except StopIteration:
                            alive.remove(g_)
                return alive
            if smp:
                self.pd_banks = list(range(8))
                rr([g_proj([0, 1, 2, 3, 4])])
                self.pd_banks = [0, 1]
                rr([g_rwkv()])
                rr([g_s5()])
            else:
                self.pd_banks = list(range(8))
                gp = g_proj([0, 1, 2, 3, 4])
                for _ in range(15):
                    next(gp)
                self.pd_banks = [0, 1, 5, 6, 7]
                gr = g_rwkv()
                rest = rr([gr, gp], until=gp)
                self.pd_banks = [0, 1]
                rr(rest + [g_s5()])
            self.pd_banks = list(range(8))
            for half in range(2):
                wt, bw = wload_t(s_wout[l, half], 512, b_swout[l])
                for cc in range(4):
                    pt, bpt = pdense()
                    for kc in range(8):
                        self.mm(pt[:, 0:N], wt[:, kc, cc * 128:(cc + 1) * 128], ACTB[:, kc, 0:N], kc == 0, kc == 7, [bw, bACTk[kc]], [bpt])
                    j = half * 4 + cc
                    self.tt(xT[:, j, 0:N], xT[:, j, 0:N], pt[:, 0:N], ALU.add, [bxT, bpt], [bxT])
            rmsnorm("norm_x", l, N)
            if not smp:
                P.dma("sp", KTl[:].rearrange("p a m -> p (a m)"), s_kt[l], reads=[b_skt[l]], writes=[bKTl])
                P.dma("sp", VMl[:].rearrange("p a m -> p (a m)"), s_vm[l], reads=[b_svm[l]], writes=[bVMl])
            for half in range(2):
                wt, bw = wload_t(s_wq[l, half], 512, b_swq[l])
                for cc in range(4):
                    pt, bpt = pdense()
                    for kc in range(8):
                        self.mm(pt[:, 0:N], wt[:, kc, cc * 128:(cc + 1) * 128], ACTB[:, kc, 0:N], kc == 0, kc == 7, [bw, bACTk[kc]], [bpt])
                    self.cp(SCRB[:, half * 4 + cc, 0:N], pt[:, 0:N], [bpt], [bSCRk[half * 4 + cc]], eng="act")
            if smp:
                smp_attn(l)
            if not smp:
                ets = {}

                def at_scores(h):
                    et, bet = ETb.next()
                    ets[h] = (et, bet)
                    for mc in range(2):
                        bk = (2 + mc) if h % 2 == 0 else mc
                        pt, bpt = PS[bk], BPS[bk]
                        for dc in range(2):
                            self.mm(pt[:, 0:N], KTl[:, 2 * h + dc, mc * 128:(mc + 1) * 128], SCRB[:, 2 * h + dc, 0:N], dc == 0, dc == 1, [bKTl, bSCRk[2 * h + dc]], [bpt])
                        self.act(et[:, mc, :], pt[:, 0:N], AF.Exp, [bpt], [bet], scale=1.0 / 16.0)

                def at_pv(h):
                    et, bet = ets.pop(h)
                    hf = (h % 2) * 256
                    pdn, bpdn = PS[4][:, hf:hf + 256], BPS[4]
                    for mc in range(2):
                        self.mm(pdn[:, 0:N], onesb[:], et[:, mc, :], mc == 0, mc == 1, [bet, bc], [bpdn])
                    rd, brd = rden.next()
                    self.act(rd[:, :], pdn[:, 0:N], AF.Ln, [bpdn], [brd])
                    self.act(rd[:, :], rd[:, :], AF.Exp, [brd], [brd], scale=-1.0)
                    pob, bpo = PS[5 + h % 2], BPS[5 + h % 2]
                    for dc in range(2):
                        po = pob[:, dc * 256:dc * 256 + 256]
                        for mc in range(2):
                            self.mm(po[:, 0:N], VMl[:, mc, (2 * h + dc) * 128:(2 * h + dc + 1) * 128], et[:, mc, :], mc == 0, mc == 1, [bVMl, bet], [bpo])
                    for dc in range(2):
                        po = pob[:, dc * 256:dc * 256 + 256]
                        self.tt(ACTB[:, 2 * h + dc, 0:N], po[:, 0:N], rd[:, :], ALU.mult, [bpo, brd], [bACTk[2 * h + dc]])
                at_scores(0)
                for h in range(4):
                    if h + 1 < 4:
                        at_scores(h + 1)
                    at_pv(h)
            for half in range(2):
                wt, bw = wload_t(s_wo[l, half], 512, b_swo[l])
                for cc in range(4):
                    pt, bpt = pdense()
                    for kc in range(8):
                        self.mm(pt[:, 0:N], wt[:, kc, cc * 128:(cc + 1) * 128], ACTB[:, kc, 0:N], kc == 0, kc == 7, [bw, bACTk[kc]], [bpt])
                    j = half * 4 + cc
                    self.tt(xT[:, j, 0:N], xT[:, j, 0:N], pt[:, 0:N], ALU.add, [bxT, bpt], [bxT])
            if smp:
                smp_store_shift(l)
            if last_tile:
                pt, bpt = PS[2], BPS[2]
                self.tr(pt[0:13, 0:128], carry[:, l, :], identf[:], [bcarry, bc], [bpt])
                self.cp(shs[0:13, :], pt[0:13, 0:128], [bpt], [bshs], eng="act")
                P.dma("sp", o_pshift[l].rearrange("(c p) -> c p", p=128), shs[0:13, :], reads=[bshs], final=True)
                pt, bpt = PS[3], BPS[3]
                for c in range(4):
                    self.tr(pt[0:64, c * 128:(c + 1) * 128], Hst[:, l, c, :], identf[:], [bH, bc], [bpt])
                self.cp(wko[:, :, :], pt[0:64, :].rearrange("p (c m) -> p c m", c=4), [bpt], [bwko], eng="act")
                P.dma("sp", o_pwkv[l].rearrange("(c h2) i j -> i c h2 j", h2=2), wko[:, :, :].rearrange("p c (h2 j) -> p c h2 j", h2=2), reads=[bwko], final=True)
                pt, bpt = PS[2], BPS[2]
                for ri in range(2):
                    self.tr(pt[0:16, ri * 128:(ri + 1) * 128], S5O[:, ri, :], identf[:], [bS5O, bc], [bpt])
                self.cp(s5o2[:, :, :], pt[0:16, 0:256].rearrange("p (a m) -> p a m", a=2), [bpt], [bs5o2], eng="act")
                P.dma("sp", o_ps5re[l].rearrange("(k g2) n -> k (g2 n)", g2=2), s5o2[:, 0, :], reads=[bs5o2], final=True)
                P.dma("sp", o_ps5im[l].rearrange("(k g2) n -> k (g2 n)", g2=2), s5o2[:, 1, :], reads=[bs5o2], final=True)

        for ti in range(NT):
            load_x(ti * N)
            if self.cfg.get("stop") != "C":
                for l in range(NL):
                    layer(l, ti == NT - 1, TN, False)
            store_y(ti * N)
        if not self.sample:
            P.emit()
            return nc
        P.emit(final=False)
        P.release(prompt_mark)
        P.barrier()
        xs = self.din("xs", [NB_S, T_S, D])
        st_shift = self.din("st_shift", [DEPTH, NB_S, SHIFT])
        st_wkv = self.din("st_wkv", [DEPTH, NB_S, 8, 64, 64])
        st_s5re = self.din("st_s5re", [DEPTH, NB_S, 2048])
        st_s5im = self.din("st_s5im", [DEPTH, NB_S, 2048])
        ck = self.din("ck", [DEPTH, NB_S, MEM, D])
        cv = self.din("cv", [DEPTH, NB_S, MEM, D])
        ys = self.dout("y_s", [NB_S, T_S, D])
        o_sshift = self.dout("s_shift", [DEPTH, NB_S, SHIFT])
        o_swkv = self.dout("s_wkv", [DEPTH, NB_S, 8, 64, 64])
        o_ss5re = self.dout("s_s5_re", [DEPTH, NB_S, 2048])
        o_ss5im = self.dout("s_s5_im", [DEPTH, NB_S, 2048])
        s_b1 = self.dscr("s_b1", [6, 64, 512])
        s_b2 = self.dscr("s_b2", [64, 512])
        bsb1, bsb2 = Buf(), Buf()
        SSH = P.sb("SSH", [128, 13, 16]); bSSH = Buf()
        NSH = P.sb("NSH", [128, 13, 16]); bNSH = Buf()
        TMS = Rot(P, "TMS", 2, [64, 6, 128])
        X6 = P.sb("X6", [128, 6, 4, 64]); bX6s = [Buf() for _ in range(3)]
        Ssm = P.sb("Ssm", [128, 64, 64]); bSlo, bShi = Buf(), Buf()
        T1s = P.sb("T1s", [128, 64, 64]); bT1lo, bT1hi = Buf(), Buf()
        sks = P.sb("sks", [128, 64]); bsk = Buf()
        Ysm = P.sb("Ysm", [128, 4, 64]); bYsm = Buf()
        ytm = P.sb("ytm", [64, 512]); bytm = Buf()
        Hs5 = P.sb("Hs5", [128, 2, 2, 16, 16]); bHs5 = Buf()
        HsT = P.sb("HsT", [128, 2, 16, 64], BF16); bHsT = Buf()
        s5t = P.sb("s5t", [128, 2, 16, 16]); bs5t = Buf()
        Kf32 = P.sb("Kf32", [128, 2, D]); bKf32 = Buf()
        KTs = P.sb("KTs", [128, 8, MEM], BF16); bKTs = Buf()
        Vs = P.sb("Vs", [128, 2, D], BF16); bVs = Buf()
        ETs = P.sb("ETs", [128, 16, 32], BF16); bETs = Buf()
        rds = P.sb("rds", [128, 16, 16]); brds = Buf()
        print("SBUF bytes/partition (sample phase):", P.sb_bytes)
        NS = NB_S * T_S
        xi, bxi = xin.next()
        for t in range(T_S):
            P.dma("sp", xi[t * 16:(t + 1) * 16, :], xs[:, t, :], writes=[bxi])
        for half in range(2):
            pt, bpt = PS[2 + half], BPS[2 + half]
            for q in range(4):
                kc = half * 4 + q
                self.tr(pt[:, q * 64:(q + 1) * 64], xi[0:64, kc * 128:(kc + 1) * 128], identf[0:64, 0:64], [bxi, bc], [bpt])
            self.cp(xT[:, half * 4:(half + 1) * 4, 0:NS], pt[:, 0:256].rearrange("p (q m) -> p q m", q=4), [bpt], [bxT], eng="act")
        for l in range(NL):
            layer(l, False, NS, True)
        self.act(SCRB[:, :, 0:NS], xT[:, :, 0:NS], AF.Square, [bxT], bSCRk)
        pt, bpt = pdense()
        for kc in range(8):
            self.mm(pt[:, 0:NS], onesb[:], SCRB[:, kc, 0:NS], kc == 0, kc == 7, [bSCRk[kc], bc], [bpt])
        self.act(rstd[:, 0:NS], pt[:, 0:NS], AF.Ln, [bpt], [brstd], bias=NORM_EPS, scale=1.0 / D)
        self.act(rstd[:, 0:NS], rstd[:, 0:NS], AF.Exp, [brstd], [brstd], scale=-0.5)
        for kc in range(8):
            self.stt(xT[:, kc, 0:NS], xT[:, kc, 0:NS], pc("norm_f", 0, kc), rstd[:, 0:NS], ALU.mult, ALU.mult, [bxT, brstd, bprm], [bxT])
        xi, bxi = xin.next()
        for half in range(2):
            pt, bpt = PS[2 + half], BPS[2 + half]
            for q in range(4):
                kc = half * 4 + q
                self.tr(pt[0:64, q * 128:(q + 1) * 128], xT[:, kc, 0:NS], identf[:], [bxT, bc], [bpt])
            self.cp(xi[0:64, half * 512:(half + 1) * 512], pt[0:64, :], [bpt], [bxi], eng="act")
        for t in range(T_S):
            P.dma("sp", ys[:, t, :], xi[t * 16:(t + 1) * 16, :], reads=[bxi], final=True)
        print("dma semaphores used:", P.nsem)
        P.emit()
        return nc


def _core_inputs(inp, core, cfg):
    TN = cfg.get("TN", 256)
    NT = cfg.get("NT", SEQ // TN)
    m = {}
    m["xp"] = np.ascontiguousarray(inp["x_prompt"][core, :NT * TN])
    m["memp"] = np.ascontiguousarray(inp["mem_prompt"][core])
    for n in ("w_in", "w_out", "wq", "wk", "wv", "wo", "w_glu", "w2", "a2", "v1", "v2", "norm_mix", "norm_x", "norm_mem",
              "mu_shift", "w0", "a0", "k_k", "k_a", "gn_w", "gn_b", "d_skip", "v0", "log_dt", "b_re", "b_im", "c_re", "c_im"):
        m[n] = inp[n]
    m["norm_f"] = inp["norm_f"].reshape(1, D)
    m["r_k"] = inp["r_k"].reshape(DEPTH, RW)
    m["lam_re"] = inp["lam_re"].reshape(DEPTH, 2048)
    m["lam_im"] = inp["lam_im"].reshape(DEPTH, 2048)
    if cfg.get("sample", True):
        bs = slice(core * NB_S, (core + 1) * NB_S)
        m["xs"] = np.ascontiguousarray(inp["x_sample"][bs])
        m["st_shift"] = np.ascontiguousarray(inp["state_shift"][:, bs])
        m["st_wkv"] = np.ascontiguousarray(inp["state_wkv"][:, bs])
        m["st_s5re"] = np.ascontiguousarray(inp["state_s5_re"][:, bs]).reshape(DEPTH, NB_S, 2048)
        m["st_s5im"] = np.ascontiguousarray(inp["state_s5_im"][:, bs]).reshape(DEPTH, NB_S, 2048)
        m["ck"] = np.ascontiguousarray(inp["cache_mem_k"][:, bs]).reshape(DEPTH, NB_S, MEM, D)
        m["cv"] = np.ascontiguousarray(inp["cache_mem_v"][:, bs]).reshape(DEPTH, NB_S, MEM, D)
    return m


_NC_CACHE = {}


def run(inp, cfg, cores=8):
    key = tuple(sorted(cfg.items()))
    if key not in _NC_CACHE:
        _NC_CACHE[key] = K(cfg).build()
    nc = _NC_CACHE[key]
    inp = {n: np.asarray(v) for n, v in inp.items()}
    in_maps = [_core_inputs(inp, c, cfg) for c in range(cores)]
    res = run_bass_kernel_spmd(nc, in_maps, core_ids=list(range(cores)))
    return res.results


def kernel(**inputs):
    cfg = {}
    res = run(inputs, cfg, 8)
    f = np.float32
    cat = lambda k: np.stack([np.asarray(r[k], dtype=f) for r in res], axis=0)
    y_prompt = cat("y_p")
    y_sample = cat("y_s").reshape(8 * NB_S, T_S, D)
    st1 = lambda k, shp: np.ascontiguousarray(np.moveaxis(cat(k), 0, 1)).reshape(shp)
    p_shift = st1("p_shift", (DEPTH, 8, SHIFT))
    p_wkv = st1("p_wkv", (DEPTH, 8, 8, 64, 64))
    p_s5_re = st1("p_s5_re", (DEPTH, 8, 32, 64))
    p_s5_im = st1("p_s5_im", (DEPTH, 8, 32, 64))
    p_mem_k = st1("p_mem_k", (DEPTH, 8, MEM, 4, 256))
    p_mem_v = st1("p_mem_v", (DEPTH, 8, MEM, 4, 256))
    s_shift = st1("s_shift", (DEPTH, 8 * NB_S, SHIFT))
    s_wkv = st1("s_wkv", (DEPTH, 8 * NB_S, 8, 64, 64))
    s_s5_re = st1("s_s5_re", (DEPTH, 8 * NB_S, 32, 64))
    s_s5_im = st1("s_s5_im", (DEPTH, 8 * NB_S, 32, 64))
    return (y_prompt, y_sample, p_shift, p_wkv, p_s5_re, p_s5_im, p_mem_k, p_mem_v, s_shift, s_wkv, s_s5_re, s_s5_im)
```
